# Optimizing a Trainium2 kernel written in Bass

```python
import math
import jax, jax.numpy as jnp
from jax import lax
import numpy as np


D_MODEL = 2048
BATCH = 2
SEQ = 4096
DEPTH = 1

GRID_W = 64
CTX_LEN = 256
HEAD_DIM = 128
N_HEADS_SWA = D_MODEL // (2 * HEAD_DIM)
N_KV_SWA = 2
GQA_GROUP = N_HEADS_SWA // N_KV_SWA
WINDOW = 128
ATT_BLOCK = 128
DIFF_V_DIM = 2 * HEAD_DIM
N_HEADS_DIFF = D_MODEL // (2 * DIFF_V_DIM)
ROPE_THETA = 10000.0
ROPE_PAIRS = HEAD_DIM // 4
N_GROUPS = 4
EXPERTS_PER_GROUP = 8
N_EXPERTS = N_GROUPS * EXPERTS_PER_GROUP
TOP_K_INNER = 2
D_EXPERT = D_MODEL // 4
MOE_BLOCK = 128
ALPHA = (2.0 * DEPTH) ** 0.25
BETA = (8.0 * DEPTH) ** -0.25
LN_EPS = 1e-6
SUBLN_EPS = 1e-5

QA_DIM = N_HEADS_SWA * HEAD_DIM
KA_DIM = N_KV_SWA * HEAD_DIM
VA_DIM = N_KV_SWA * HEAD_DIM
QB_DIM = N_HEADS_DIFF * 2 * HEAD_DIM
KB_DIM = N_HEADS_DIFF * 2 * HEAD_DIM
VB_DIM = N_HEADS_DIFF * DIFF_V_DIM
QKV_DIM = QA_DIM + KA_DIM + VA_DIM + QB_DIM + KB_DIM + VB_DIM
D_MIX = N_HEADS_SWA * HEAD_DIM + N_HEADS_DIFF * DIFF_V_DIM
SPLIT_POINTS = (QA_DIM, QA_DIM + KA_DIM, QA_DIM + KA_DIM + VA_DIM,
                QA_DIM + KA_DIM + VA_DIM + QB_DIM,
                QA_DIM + KA_DIM + VA_DIM + QB_DIM + KB_DIM)

kernel_name = "hymba_swa_diffattn_hiermoe_dit_block"


def layer_norm(x, g, b):
    xf = x.astype(jnp.float32)
    mu = jnp.mean(xf, axis=-1, keepdims=True)
    var = jnp.mean(jnp.square(xf - mu), axis=-1, keepdims=True)
    y = (xf - mu) * lax.rsqrt(var + LN_EPS)
    return (y * g.astype(jnp.float32) + b.astype(jnp.float32)).astype(x.dtype)


def diff_out_norm(o, g, lam_init):
    of = o.astype(jnp.float32)
    y = of * lax.rsqrt(jnp.mean(jnp.square(of), axis=-1, keepdims=True) + SUBLN_EPS)
    return (y * g.astype(jnp.float32) * (1.0 - lam_init)).astype(o.dtype)


def rope_2d(x, cos_r, sin_r, cos_c, sin_c):
    extra = x.ndim - 3
    shp = lambda t: t.reshape(t.shape[:1] + (1,) * extra + t.shape[1:])
    cr, sr, cc, sc = shp(cos_r), shp(sin_r), shp(cos_c), shp(sin_c)
    r1, r2, c1, c2 = jnp.split(x, 4, axis=-1)
    return jnp.concatenate([r1 * cr - r2 * sr, r2 * cr + r1 * sr,
                            c1 * cc - c2 * sc, c2 * cc + c1 * sc], axis=-1)


def split_columns(p):
    lead = p.shape[:-1]
    qa, ka, va, qb, kb, vb = jnp.split(p, SPLIT_POINTS, axis=-1)
    return (qa.reshape(lead + (N_HEADS_SWA, HEAD_DIM)),
            ka.reshape(lead + (N_KV_SWA, HEAD_DIM)),
            va.reshape(lead + (N_KV_SWA, HEAD_DIM)),
            qb.reshape(lead + (N_HEADS_DIFF, 2, HEAD_DIM)),
            kb.reshape(lead + (N_HEADS_DIFF, 2, HEAD_DIM)),
            vb.reshape(lead + (N_HEADS_DIFF, DIFF_V_DIM)))


def softmax_with_sink(scores, sink):
    m = jnp.maximum(jnp.max(scores, axis=-1, keepdims=True), sink)
    e = jnp.exp(scores - m)
    return e / (jnp.sum(e, axis=-1, keepdims=True) + jnp.exp(sink - m))


def windowed_gqa_sink(q, k, v, k_ctx, v_ctx, sink):
    B, S = q.shape[:2]
    nb = S // ATT_BLOCK
    scale = HEAD_DIM ** -0.5
    qb = q.reshape(B, nb, ATT_BLOCK, N_KV_SWA, GQA_GROUP, HEAD_DIM)

    def bands(t):
        tp = jnp.pad(t, ((0, 0), (ATT_BLOCK, ATT_BLOCK), (0, 0), (0, 0)))
        tp = tp.reshape(B, nb + 2, ATT_BLOCK, N_KV_SWA, HEAD_DIM)
        return jnp.concatenate([tp[:, :-2], tp[:, 1:-1], tp[:, 2:]], axis=2)

    kb, vb = bands(k), bands(v)
    s_loc = jnp.einsum('bnqkgd,bnjkd->bkgnqj', qb, kb).astype(jnp.float32) * scale
    s_ctx = jnp.einsum('bnqkgd,bckd->bkgnqc', qb, k_ctx).astype(jnp.float32) * scale
    blk = jnp.arange(nb)[:, None, None]
    qpos = blk * ATT_BLOCK + jnp.arange(ATT_BLOCK)[None, :, None]
    kpos = (blk - 1) * ATT_BLOCK + jnp.arange(3 * ATT_BLOCK)[None, None, :]
    valid = (jnp.abs(kpos - qpos) <= WINDOW) & (kpos >= 0) & (kpos < S)
    s_loc = jnp.where(valid, s_loc, -jnp.inf)
    sk = sink.astype(jnp.float32).reshape(1, N_KV_SWA, GQA_GROUP, 1, 1, 1)
    p = softmax_with_sink(jnp.concatenate([s_loc, s_ctx], axis=-1), sk).astype(v.dtype)
    p_loc, p_ctx = p[..., :3 * ATT_BLOCK], p[..., 3 * ATT_BLOCK:]
    out = (jnp.einsum('bkgnqj,bnjkd->bnqkgd', p_loc, vb)
           + jnp.einsum('bkgnqc,bckd->bnqkgd', p_ctx, v_ctx))
    return out.reshape(B, S, N_HEADS_SWA * HEAD_DIM)


def context_gqa_sink(q, k, v, sink):
    B, C = q.shape[:2]
    qg = q.reshape(B, C, N_KV_SWA, GQA_GROUP, HEAD_DIM)
    s = jnp.einsum('bqkgd,bckd->bkgqc', qg, k).astype(jnp.float32) * HEAD_DIM ** -0.5
    sk = sink.astype(jnp.float32).reshape(1, N_KV_SWA, GQA_GROUP, 1, 1)
    p = softmax_with_sink(s, sk).astype(v.dtype)
    return jnp.einsum('bkgqc,bckd->bqkgd', p, v).reshape(B, C, N_HEADS_SWA * HEAD_DIM)


def diff_attend(q, k, v, lam):
    s = jnp.einsum('bqhrd,bkhrd->bhrqk', q, k).astype(jnp.float32) * HEAD_DIM ** -0.5
    p = jax.nn.softmax(s, axis=-1)
    a = p[:, :, 0] - lam * p[:, :, 1]
    return jnp.einsum('bhqk,bkhe->bqhe', a.astype(v.dtype), v)


def latent_diff_attention(q, k_all, v_all, lam):
    B, S = q.shape[:2]
    nb = S // ATT_BLOCK
    qs = jnp.moveaxis(q.reshape(B, nb, ATT_BLOCK, N_HEADS_DIFF, 2, HEAD_DIM), 1, 0)
    out = lax.map(lambda qblk: diff_attend(qblk, k_all, v_all, lam), qs)
    return jnp.moveaxis(out, 0, 1).reshape(B, S, N_HEADS_DIFF, DIFF_V_DIM)


def hier_moe(h, w_rg, b_rg, w_re, b_re, w_gate, w_up, w_down):
    n_tok, d = h.shape
    p_group = jax.nn.softmax((h @ w_rg + b_rg).astype(jnp.float32), axis=-1)
    g_idx = jnp.argmax(p_group, axis=-1).astype(jnp.int32)
    p_sel = jnp.take_along_axis(p_group, g_idx[:, None], axis=-1)
    logit_e = (h @ w_re + b_re).astype(jnp.float32).reshape(n_tok, N_GROUPS, EXPERTS_PER_GROUP)
    logit_in = jnp.take_along_axis(logit_e, g_idx[:, None, None], axis=1)[:, 0]
    top_logit, top_local = lax.top_k(logit_in, TOP_K_INNER)
    weights = (p_sel * jax.nn.softmax(top_logit, axis=-1)).astype(h.dtype)
    expert_id = g_idx[:, None] * EXPERTS_PER_GROUP + top_local.astype(jnp.int32)

    n_assign = n_tok * TOP_K_INNER
    e_flat = expert_id.reshape(-1)
    tok_flat = jnp.repeat(jnp.arange(n_tok, dtype=jnp.int32), TOP_K_INNER)
    w_flat = weights.reshape(-1)
    order = jnp.argsort(e_flat)
    e_s, tok_s, w_s = e_flat[order], tok_flat[order], w_flat[order]
    counts = jnp.zeros((N_EXPERTS,), jnp.int32).at[e_flat].add(1)
    start = jnp.cumsum(counts) - counts
    padded = (counts + MOE_BLOCK - 1) // MOE_BLOCK * MOE_BLOCK
    pend = jnp.cumsum(padded)
    pstart = pend - padded
    dest = pstart[e_s] + (jnp.arange(n_assign, dtype=jnp.int32) - start[e_s])
    cap = n_assign + N_EXPERTS * MOE_BLOCK
    n_blocks = cap // MOE_BLOCK
    slot_tok = jnp.full((cap,), n_tok, jnp.int32).at[dest].set(tok_s)
    slot_w = jnp.zeros((cap,), h.dtype).at[dest].set(w_s)
    block_start = jnp.arange(n_blocks, dtype=jnp.int32) * MOE_BLOCK
    block_expert = jnp.minimum(jnp.searchsorted(pend, block_start, side='right'),
                               N_EXPERTS - 1).astype(jnp.int32)
    h_pad = jnp.concatenate([h, jnp.zeros((1, d), h.dtype)], axis=0)

    def expert_block(args):
        toks, e = args
        xb = h_pad[toks]
        return (jax.nn.silu(xb @ w_gate[e]) * (xb @ w_up[e])) @ w_down[e]

    y_slots = lax.map(expert_block, (slot_tok.reshape(n_blocks, MOE_BLOCK), block_expert))
    y_slots = y_slots.reshape(cap, d) * slot_w[:, None]
    return jax.ops.segment_sum(y_slots, slot_tok, num_segments=n_tok + 1)[:n_tok]


def setup_inputs(seed: int = 0) -> dict:
    key = jax.random.key(seed)
    ks = jax.random.split(key, 26)
    f32 = jnp.float32
    nrm = lambda k, shape, s: jax.random.normal(k, shape, f32) * s
    D = D_MODEL
    return {
        "x": nrm(ks[0], (BATCH, SEQ, D), 1.0),
        "c": nrm(ks[1], (BATCH, D), 1.0),
        "ctx": nrm(ks[2], (BATCH, CTX_LEN, D), 1.0),
        "c_ctx": nrm(ks[3], (D,), 1.0),
        "w_ada": nrm(ks[4], (DEPTH, D, 6 * D), D ** -0.5),
        "b_ada": nrm(ks[5], (DEPTH, 6 * D), 0.02),
        "w_in": nrm(ks[6], (DEPTH, D, QKV_DIM), D ** -0.5),
        "w_out": nrm(ks[7], (DEPTH, D_MIX, D), BETA * D_MIX ** -0.5),
        "sink": nrm(ks[8], (DEPTH, N_HEADS_SWA), 0.5),
        "lam_q1": nrm(ks[9], (DEPTH, HEAD_DIM), 0.1),
        "lam_k1": nrm(ks[10], (DEPTH, HEAD_DIM), 0.1),
        "lam_q2": nrm(ks[11], (DEPTH, HEAD_DIM), 0.1),
        "lam_k2": nrm(ks[12], (DEPTH, HEAD_DIM), 0.1),
        "subln_g": 1.0 + nrm(ks[13], (DEPTH, DIFF_V_DIM), 0.02),
        "ln1_g": 1.0 + nrm(ks[14], (DEPTH, D), 0.02),
        "ln1_b": nrm(ks[15], (DEPTH, D), 0.02),
        "w_router_group": nrm(ks[16], (DEPTH, D, N_GROUPS), D ** -0.5),
        "b_router_group": nrm(ks[17], (DEPTH, N_GROUPS), 0.01),
        "w_router_expert": nrm(ks[18], (DEPTH, D, N_EXPERTS), D ** -0.5),
        "b_router_expert": nrm(ks[19], (DEPTH, N_EXPERTS), 0.01),
        "w_gate": nrm(ks[20], (DEPTH, N_EXPERTS, D, D_EXPERT), D ** -0.5),
        "w_up": nrm(ks[21], (DEPTH, N_EXPERTS, D, D_EXPERT), D ** -0.5),
        "w_down": nrm(ks[22], (DEPTH, N_EXPERTS, D_EXPERT, D), BETA * D_EXPERT ** -0.5),
        "ln2_g": 1.0 + nrm(ks[23], (DEPTH, D), 0.02),
        "ln2_b": nrm(ks[24], (DEPTH, D), 0.02),
    }


def reference(x, c, ctx, c_ctx, w_ada, b_ada, w_in, w_out, sink, lam_q1, lam_k1,
              lam_q2, lam_k2, subln_g, ln1_g, ln1_b, w_router_group, b_router_group,
              w_router_expert, b_router_expert, w_gate, w_up, w_down, ln2_g, ln2_b):
    B, S, D = x.shape
    rows = S // GRID_W
    row = jnp.repeat(jnp.arange(rows), GRID_W).astype(jnp.float32)
    col = jnp.tile(jnp.arange(GRID_W), rows).astype(jnp.float32)
    inv_freq = ROPE_THETA ** (-jnp.arange(ROPE_PAIRS, dtype=jnp.float32) / ROPE_PAIRS)
    ang_r, ang_c = row[:, None] * inv_freq, col[:, None] * inv_freq
    rope = (jnp.cos(ang_r).astype(x.dtype), jnp.sin(ang_r).astype(x.dtype),
            jnp.cos(ang_c).astype(x.dtype), jnp.sin(ang_c).astype(x.dtype))
    ctx_s = ctx
    for i in range(DEPTH):
        lam_init = 0.8 - 0.6 * math.exp(-0.3 * i)
        lam = (jnp.exp(jnp.sum(lam_q1[i].astype(jnp.float32) * lam_k1[i].astype(jnp.float32)))
               - jnp.exp(jnp.sum(lam_q2[i].astype(jnp.float32) * lam_k2[i].astype(jnp.float32)))
               + lam_init)
        sh1, sc1, g1, sh2, sc2, g2 = jnp.split(jax.nn.silu(c) @ w_ada[i] + b_ada[i], 6, axis=-1)
        sh1c, sc1c, g1c, sh2c, sc2c, g2c = jnp.split(
            jax.nn.silu(c_ctx) @ w_ada[i] + b_ada[i], 6, axis=-1)

        h = x * (1.0 + sc1[:, None]) + sh1[:, None]
        hc = ctx_s * (1.0 + sc1c) + sh1c
        qa, ka, va, qb, kb, vb = split_columns(h @ w_in[i])
        qac, kac, vac, qbc, kbc, vbc = split_columns(hc @ w_in[i])
        qa, ka, qb, kb = (rope_2d(t, *rope) for t in (qa, ka, qb, kb))
        out_a = windowed_gqa_sink(qa, ka, va, kac, vac, sink[i])
        k_all = jnp.concatenate([kb, kbc], axis=1)
        v_all = jnp.concatenate([vb, vbc], axis=1)
        out_b = diff_out_norm(latent_diff_attention(qb, k_all, v_all, lam), subln_g[i], lam_init)
        mix = jnp.concatenate([out_a, out_b.reshape(B, S, VB_DIM)], axis=-1) @ w_out[i]
        x = layer_norm(ALPHA * x + g1[:, None] * mix, ln1_g[i], ln1_b[i])

        h2 = x * (1.0 + sc2[:, None]) + sh2[:, None]
        y = hier_moe(h2.reshape(B * S, D), w_router_group[i], b_router_group[i],
                     w_router_expert[i], b_router_expert[i],
                     w_gate[i], w_up[i], w_down[i]).reshape(B, S, D)

        if i + 1 < DEPTH:
            C = ctx_s.shape[1]
            oc_b = diff_out_norm(diff_attend(qbc, kbc, vbc, lam), subln_g[i], lam_init)
            mixc = jnp.concatenate([context_gqa_sink(qac, kac, vac, sink[i]),
                                    oc_b.reshape(B, C, VB_DIM)], axis=-1) @ w_out[i]
            ctx_s = layer_norm(ALPHA * ctx_s + g1c * mixc, ln1_g[i], ln1_b[i])
            h2c = ctx_s * (1.0 + sc2c) + sh2c
            yc = hier_moe(h2c.reshape(B * C, D), w_router_group[i], b_router_group[i],
                          w_router_expert[i], b_router_expert[i],
                          w_gate[i], w_up[i], w_down[i]).reshape(B, C, D)
            ctx_s = layer_norm(ALPHA * ctx_s + g2c * yc, ln2_g[i], ln2_b[i])

        x = layer_norm(ALPHA * x + g2[:, None] * y, ln2_g[i], ln2_b[i])
    return x
```

```python
import contextlib
import numpy as np
import ml_dtypes
import concourse.bass as bass
import concourse.mybir as mybir
from concourse.bass_utils import run_bass_kernel_spmd

F32 = mybir.dt.float32
BF16 = mybir.dt.bfloat16
AF = mybir.ActivationFunctionType
ALU = mybir.AluOpType
AX = mybir.AxisListType

D = 2048
SEQ = 4096
NCTX = 256
NTOK = SEQ + NCTX
OWN = 1024
NEXP = 32
SCALE = 128 ** -0.5
ALPHA = 2.0 ** 0.25
LAM_INIT = 0.2
LN_EPS = 1e-6
SUBLN_EPS = 1e-5

ENGS = ("pe", "act", "dve", "pool", "sp")
SEM_CHUNK = 8000


class Sched:
    def __init__(self, nc, stack, n_dma_sems=8, n_chunks=6):
        self.nc = nc
        self.ops = []
        self.n_dma_sems = n_dma_sems
        self.sems = {}
        for e in ENGS:
            if e == "sp":
                continue
            for k in range(n_chunks):
                self.sems[(e, k)] = stack.enter_context(nc.semaphore("c_%s_%d" % (e, k)))
        for e in ("sp", "pool"):
            for k in range(n_dma_sems):
                self.sems[("d", e, k)] = stack.enter_context(nc.semaphore("d_%s_%d" % (e, k)))
        self.cnt = {e: 0 for e in ENGS}
        self.dma_rr = {e: 0 for e in ENGS}
        self.dma_cnt = {}
        self.waited = {e: {} for e in ENGS}
        self.n_total = 0

    def op(self, eng, fn, reads=(), writes=()):
        self.ops.append(dict(eng=eng, fn=fn, reads=tuple(reads), writes=tuple(writes), dma=False))

    def dma(self, eng, out, in_, reads=(), writes=(), **kw):
        def fn(e, out=out, in_=in_, kw=kw):
            return e.dma_start(out=out, in_=in_, **kw)
        self.ops.append(dict(eng=eng, fn=fn, reads=tuple(reads), writes=tuple(writes), dma=True))

    def flush(self):
        nc = self.nc
        ops = self.ops
        self.ops = []
        self.n_total += len(ops)
        last_w = {}
        readers = {}
        last_reader = {}
        for i, o in enumerate(ops):
            deps = set()
            for r in o["reads"]:
                if r in last_w:
                    deps.add(last_w[r])
                if isinstance(r, str) and r.startswith("ps"):
                    lr = last_reader.get(r)
                    if lr is not None and ops[lr]["eng"] != o["eng"]:
                        deps.add(lr)
                    last_reader[r] = i
            for w in o["writes"]:
                if w in last_w:
                    deps.add(last_w[w])
                for rd in readers.get(w, ()):
                    deps.add(rd)
            deps.discard(i)
            o["deps"] = deps
            for w in o["writes"]:
                last_w[w] = i
                readers[w] = []
                last_reader.pop(w, None)
            for r in o["reads"]:
                if r not in o["writes"]:
                    readers.setdefault(r, []).append(i)
        signal = [False] * len(ops)
        for i, o in enumerate(ops):
            nd = set()
            for d in o["deps"]:
                od = ops[d]
                if (not od["dma"]) and (not o["dma"]) and od["eng"] == "pe" and o["eng"] == "pe":
                    continue
                nd.add(d)
                if not od["dma"]:
                    signal[d] = True
            o["deps"] = nd
        last_comp = {}
        for i, o in enumerate(ops):
            if not o["dma"] and o["fn"] is not None:
                last_comp[o["eng"]] = i
        for e, i in last_comp.items():
            signal[i] = True
        for i, o in enumerate(ops):
            if o["dma"]:
                q = o["eng"]
                s = ("d", q, self.dma_rr[q] % self.n_dma_sems)
                self.dma_rr[q] += 1
                prev = self.dma_cnt.get(s, 0)
                o["sem"] = s
                o["prev"] = prev
                o["val"] = prev + 16
                self.dma_cnt[s] = prev + 16
            elif signal[i]:
                c = self.cnt[o["eng"]]
                self.cnt[o["eng"]] = c + 1
                o["sem"] = (o["eng"], c // SEM_CHUNK)
                o["val"] = c % SEM_CHUNK + 1
        per_eng = {e: [] for e in ENGS}
        waited = self.waited
        for i, o in enumerate(ops):
            F = o["eng"]
            need = {}
            for d in o["deps"]:
                od = ops[d]
                s, v = od["sem"], od["val"]
                if v > need.get(s, 0):
                    need[s] = v
            if o["dma"] and o["prev"] > 0:
                s = o["sem"]
                if o["prev"] > need.get(s, 0):
                    need[s] = o["prev"]
            waits = []
            for s, v in need.items():
                if waited[F].get(s, 0) >= v:
                    continue
                waited[F][s] = v
                waits.append((s, v))
            per_eng[F].append((waits, o, signal[i]))
        final = {}
        for e, i in last_comp.items():
            final[ops[i]["sem"]] = ops[i]["val"]
        for s, v in self.dma_cnt.items():
            final[s] = v
        for F in ENGS:
            waits = []
            for s, v in final.items():
                if waited[F].get(s, 0) >= v:
                    continue
                waited[F][s] = v
                waits.append((s, v))
            per_eng[F].append((waits, dict(fn=None, dma=False), False))
        sems = self.sems
        with nc.Block() as block:
            def run(engname):
                def body(eng):
                    for waits, o, sig in per_eng[engname]:
                        for s, v in waits:
                            eng.wait_ge(sems[s], v)
                        if o["fn"] is None:
                            continue
                        ins = o["fn"](eng)
                        if o["dma"]:
                            ins.then_inc(sems[o["sem"]], 16)
                        elif sig:
                            ins.then_inc(sems[o["sem"]], 1)
                return body

            block.tensor(run("pe"))
            block.scalar(run("act"))
            block.vector(run("dve"))
            block.gpsimd(run("pool"))
            block.sync(run("sp"))


def build(dbg=False, n_exp=NEXP):
    nc = bass.Bass("TRN2", target_bir_lowering=False)

    def din(name, shape, dt=F32):
        return nc.dram_tensor(name, list(shape), dt, kind="ExternalInput").ap()

    def dscr(name, shape, dt):
        if dbg:
            return nc.dram_tensor(name, list(shape), dt, kind="ExternalOutput").ap()
        return nc.dram_tensor(name, list(shape), dt).ap()

    xs = din("xs", [SEQ, D])
    ctxs = din("ctxs", [NCTX, D])
    cs_in = din("cs", [128, 16, 2])
    w_ada_p = din("w_ada_p", [24, 128, 16, 512])
    b_ada_fm = din("b_ada_fm", [128, 96])
    b_ada_row = din("b_ada_row", [1, 6 * D])
    w_in_p = din("w_in_p", [9, 128, 16, 512])
    w_out_p = din("w_out_p", [128, 16, D])
    w_r_in = din("w_r", [128, 16, 36])
    b_r_in = din("b_r", [1, 36])
    wg_p = din("wg_p", [NEXP, 128, 16, 512])
    wu_p = din("wu_p", [NEXP, 128, 16, 512])
    wd_p = din("wd_p", [NEXP, 128, 4, D])
    sink_in = din("sink", [1, 8])
    lamv_in = din("lamv", [1, 512])
    subg_in = din("subln_g", [1, 256])
    ln1g_in = din("ln1_g", [1, D])
    ln1b_in = din("ln1_b", [1, D])
    ln2g_in = din("ln2_g", [1, D])
    ln2b_in = din("ln2_b", [1, D])
    ropeC_in = din("ropeC", [128, SEQ])
    ropeS_in = din("ropeS", [128, SEQ])
    identb_in = din("ident_b", [128, 128], BF16)
    identf_in = din("ident_f", [128, 128])
    pswap_in = din("pswap", [128, 128], BF16)
    masks_in = din("masks", [128, 4, 128], BF16)
    out = nc.dram_tensor("out", [OWN, D], F32, kind="ExternalOutput").ap()

    hT_d = dscr("hT_d", [16, 128, NTOK], BF16)
    QTA_d = dscr("QTA_d", [128, 8, OWN], BF16)
    KTA_d = dscr("KTA_d", [128, 2, 1536], BF16)
    VA_d = dscr("VA_d", [1536, 256], BF16)
    QTB_d = dscr("QTB_d", [128, 8, OWN], BF16)
    KTB_d = dscr("KTB_d", [128, 8, NTOK], BF16)
    VB_d = dscr("VB_d", [NTOK, 1024], BF16)
    AOT_d = dscr("AOT_d", [16, 128, OWN], BF16)
    G_d = dscr("G_d", [2, 128, D], F32)
    X1_d = dscr("X1_d", [OWN, D], F32)
    Wt_d = dscr("Wt_d", [32, OWN], F32)

    with contextlib.ExitStack() as top:
        S = Sched(nc, top)
        banks = [nc.alloc_psum_tensor("bank%d" % k, [128, 512], F32) for k in range(8)]
        PS = ["ps%d" % k for k in range(8)]

        def sbt(stack, name, shape, dt):
            return stack.enter_context(nc.sbuf_tensor("s_" + name, list(shape), dt))

        ident_b = sbt(top, "ident_b", [128, 128], BF16)
        ident_f = sbt(top, "ident_f", [128, 128], F32)
        pswap = sbt(top, "pswap", [128, 128], BF16)
        ones_b = sbt(top, "ones_b", [128, 128], BF16)
        masks = sbt(top, "masks", [128, 4, 128], BF16)
        modFM = sbt(top, "modFM", [128, 96, 2], F32)
        eps_ln = sbt(top, "eps_ln", [128, 1], F32)
        eps_sub = sbt(top, "eps_sub", [128, 1], F32)
        cs_b = sbt(top, "cs_b", [128, 16, 2], BF16)
        cs_s = sbt(top, "cs_s", [128, 16, 2], F32)
        bfm = sbt(top, "bfm", [128, 96], F32)
        ones_f = sbt(top, "ones_f", [128, 128], F32)

        S.dma("sp", ident_b[:], identb_in, writes=["ident_b"])
        S.dma("sp", ident_f[:], identf_in, writes=["ident_f"])
        S.dma("sp", pswap[:], pswap_in, writes=["pswap"])
        S.dma("sp", masks[:], masks_in, writes=["masks"])
        S.op("dve", lambda e: e.memset(ones_b[:], 1.0), writes=["ones_b"])
        S.op("dve", lambda e: e.memset(ones_f[:], 1.0), writes=["ones_f"])
        S.op("dve", lambda e: e.memset(eps_ln[:], LN_EPS), writes=["eps_ln"])
        S.op("dve", lambda e: e.memset(eps_sub[:], SUBLN_EPS), writes=["eps_sub"])

        _uid = [0]

        def uq(tag):
            _uid[0] += 1
            return (tag, _uid[0])

        def mm(outap, lhsT, rhs, start, stop, reads, writes):
            S.op("pe", lambda e: e.matmul(outap, lhsT=lhsT, rhs=rhs, start=start, stop=stop),
                 reads=reads, writes=writes)

        def ada_panel(j, wpan, pfm_ap, psres):
            buf = wpan[j % 2]
            wres = "wpan%d" % (j % 2)
            S.dma("pool", buf[:], w_ada_p[j], writes=[wres])
            for cc in range(4):
                col = j * 4 + cc
                for kc in range(16):
                    mm(pfm_ap[:, col, :], buf[:, kc, cc * 128:(cc + 1) * 128], cs_b[:, kc, :], kc == 0, kc == 15,
                       [wres, "cs_b"], [psres])

        with contextlib.ExitStack() as ph:
            cs_f = sbt(ph, "cs_f", [128, 16, 2], F32)
            wpan = [sbt(ph, "wpan%d" % i, [128, 16, 512], BF16) for i in range(2)]
            S.dma("sp", cs_f[:], cs_in, writes=["cs_f"])
            S.dma("sp", bfm[:], b_ada_fm, writes=["bfm"])
            S.op("act", lambda e: e.activation(out=cs_s[:], in_=cs_f[:], func=AF.Silu), reads=["cs_f"], writes=["cs_s"])
            S.op("dve", lambda e: e.tensor_copy(out=cs_b[:], in_=cs_s[:]), reads=["cs_s"], writes=["cs_b"])
            pfm = banks[0][:, 0:192].rearrange("p (c j) -> p c j", j=2)
            for j in range(8):
                ada_panel(j, wpan, pfm, PS[0])
            S.op("dve", lambda e: e.tensor_tensor(
                out=modFM[:, 0:32, :], in0=pfm[:, 0:32, :],
                in1=bfm[:, 0:32].unsqueeze(2).to_broadcast([128, 32, 2]), op=ALU.add),
                reads=[PS[0], "bfm"], writes=["modFM"])
            S.op("dve", lambda e: e.tensor_scalar_add(out=modFM[:, 16:32, :], in0=modFM[:, 16:32, :], scalar1=1.0),
                 reads=["modFM"], writes=["modFM"])
            S.flush()

        with contextlib.ExitStack() as ph23:
            KT0 = sbt(ph23, "KT0", [128, 2, NTOK], BF16)
            V0 = sbt(ph23, "V0", [128, 34, 257], BF16)
            QT0 = sbt(ph23, "QT0", [128, 2, OWN], BF16)
            with contextlib.ExitStack() as ph12:
                ropeC = sbt(ph12, "ropeC", [128, SEQ], F32)
                ropeS = sbt(ph12, "ropeS", [128, SEQ], F32)
                S.dma("sp", ropeC[:], ropeC_in, writes=["ropeC"])
                S.dma("sp", ropeS[:], ropeS_in, writes=["ropeS"])
                wp = [sbt(ph12, "wp%d" % i, [128, 16, 512], BF16) for i in range(2)]
                hT = [sbt(ph12, "hT%d" % i, [128, 16, 512], BF16) for i in range(2)]
                with contextlib.ExitStack() as ph:
                    xb = [sbt(ph, "xb%d" % i, [128, D], BF16) for i in range(3)]
                    hTt = [sbt(ph, "hTt%d" % i, [128, 16, 512], BF16) for i in range(2)]
                    for t in range(34):
                        src = xs[t * 128:(t + 1) * 128, :] if t < 32 else ctxs[(t - 32) * 128:(t - 31) * 128, :]
                        jj = 0 if t < 32 else 1
                        xr = "xb%d" % (t % 3)
                        xbt = xb[t % 3]
                        S.dma("pool", xbt[:], src, writes=[xr])
                        g, sub = t // 4, t % 4
                        hb = hTt[g % 2]
                        kA, kB = (t % 2) * 2, (t % 2) * 2 + 1
                        pa = banks[kA][:].bitcast(BF16).rearrange("p (k t) -> p k t", t=128)
                        pb = banks[kB][:].bitcast(BF16).rearrange("p (k t) -> p k t", t=128)
                        for kc in range(16):
                            pp, kk = (pa, kA) if kc < 8 else (pb, kB)
                            S.op("pe", lambda e, pp=pp, kc=kc, xbt=xbt: e.transpose(pp[:, kc % 8, :], xbt[:, kc * 128:(kc + 1) * 128], ident_b[:]),
                                 reads=[xr, "ident_b"], writes=[PS[kk]])
                        hres_a = ("hTt", g % 2, sub, 0)
                        hres_b = ("hTt", g % 2, sub, 1)
                        for kc in range(8):
                            S.op("act", lambda e, kc=kc, hb=hb, pa=pa, sub=sub, jj=jj: e.activation(
                                out=hb[:, kc, sub * 128:(sub + 1) * 128], in_=pa[:, kc, :], func=AF.Identity,
                                bias=modFM[:, kc, jj:jj + 1], scale=modFM[:, 16 + kc, jj:jj + 1]),
                                reads=[PS[kA], "modFM"], writes=[hres_a])
                        for kc in range(8, 16):
                            S.op("dve", lambda e, kc=kc, hb=hb, pb=pb, sub=sub, jj=jj: e.tensor_scalar(
                                out=hb[:, kc, sub * 128:(sub + 1) * 128], in0=pb[:, kc - 8, :],
                                scalar1=modFM[:, 16 + kc, jj:jj + 1], scalar2=modFM[:, kc, jj:jj + 1],
                                op0=ALU.mult, op1=ALU.add),
                                reads=[PS[kB], "modFM"], writes=[hres_b])
                        if sub == 3 or t == 33:
                            ntok = (sub + 1) * 128
                            base = g * 512
                            S.dma("sp", hT_d[:, :, base:base + ntok].rearrange("k p t -> p k t"), hb[:, :, 0:ntok],
                                  reads=[("hTt", g % 2, s_, ab) for s_ in range(sub + 1) for ab in (0, 1)],
                                  writes=["hT_d_g0" if g == 0 else uq("hT_d")])
                    S.dma("pool", wp[0][:], w_in_p[0], writes=["wp0_pre"])
                    S.dma("sp", hT[0][:, :, 0:512], hT_d[:, :, 0:512].rearrange("k p t -> p k t"), reads=["hT_d_g0"], writes=["hT0_pre"])
                    S.flush()

                with contextlib.ExitStack() as ph:
                    wp = wp + [sbt(ph, "wp%d" % i, [128, 16, 512], BF16) for i in (2, 3)]
                    xb16 = [sbt(ph, "xb16_%d" % i, [128, 512], BF16) for i in range(2)]
                    t1 = [sbt(ph, "t1_%d" % i, [128, 512], F32) for i in range(2)]
                    t2 = [sbt(ph, "t2_%d" % i, [128, 512], F32) for i in range(2)]
                    so = [sbt(ph, "so%d" % i, [128, 4, 512], BF16) for i in range(2)]
                    sv = [sbt(ph, "sv%d" % i, [128, 512], BF16) for i in range(2)]
                    LT = [(i * 512, 512, True) for i in range(8)]
                    HALO = (1024, 256, True)
                    CTX = (SEQ, 256, False)
                    plan = [
                        (0, "FM", QTA_d, 0, LT[0:2]), (1, "FM", QTA_d, 4, LT[0:2]),
                        (2, "KAVA", None, 0, [LT[0], LT[1], HALO, CTX]),
                        (3, "FM", QTB_d, 0, LT[0:2]), (4, "FM", QTB_d, 4, LT[0:2]),
                        (5, "FM", KTB_d, 0, LT + [CTX]), (6, "FM", KTB_d, 4, LT + [CTX]),
                        (7, "TM", VB_d, 0, LT + [CTX]), (8, "TM", VB_d, 512, LT + [CTX]),
                    ]
                    items = []
                    hcnt = 0
                    for pi, (pn, kind, dest, slot0, tiles) in enumerate(plan[0:5]):
                        for ti, tl in enumerate(tiles):
                            items.append(dict(pn=pn, kind=kind, dest=dest, slot0=slot0, tile=tl, wb=pi % 2, load_w=(ti == 0), hb=hcnt % 2, load_h=True))
                            hcnt += 1
                    wbmap = {5: 2, 6: 3, 7: 1, 8: 0}
                    for ti, tl in enumerate(LT + [CTX]):
                        for k_, pidx in enumerate((5, 6, 7)):
                            pn, kind, dest, slot0, _ = plan[pidx]
                            items.append(dict(pn=pn, kind=kind, dest=dest, slot0=slot0, tile=tl, wb=wbmap[pidx], load_w=(ti == 0), hb=hcnt % 2, load_h=(k_ == 0)))
                        hcnt += 1
                    for ti, tl in enumerate(LT + [CTX]):
                        pn, kind, dest, slot0, _ = plan[8]
                        items.append(dict(pn=pn, kind=kind, dest=dest, slot0=slot0, tile=tl, wb=wbmap[8], load_w=(ti == 0), hb=hcnt % 2, load_h=True))
                        hcnt += 1
                    cst = dict(so=0, fm=0, tm=0)
                    stores = {pn_: [] for pn_ in range(9)}
                    h_loads = [i for i, it_ in enumerate(items) if it_["load_h"]]
                    hptr = [1]
                    tile_ord = []
                    for it_ in items:
                        tile_ord.append((tile_ord[-1] if tile_ord else -1) + (1 if it_["load_h"] else 0))

                    def load_item(i):
                        it_ = items[i]
                        if it_["load_w"] and i > 0:
                            S.dma("pool", wp[it_["wb"]][:], w_in_p[it_["pn"]], writes=["wp%d" % it_["wb"]])
                        while hptr[0] < len(h_loads) and i >= 1 and tile_ord[i - 1] >= hptr[0] - 1:
                            j_ = h_loads[hptr[0]]
                            hptr[0] += 1
                            start_, ntok_, _l = items[j_]["tile"]
                            hb_ = items[j_]["hb"]
                            S.dma("sp", hT[hb_][:, :, 0:ntok_], hT_d[:, :, start_:start_ + ntok_].rearrange("k p t -> p k t"),
                                  writes=["hT%d" % hb_])

                    def compute_item(i):
                        it_ = items[i]
                        pn, kind, dest, slot0 = it_["pn"], it_["kind"], it_["dest"], it_["slot0"]
                        start, ntok, latent = it_["tile"]
                        wbuf, wres = wp[it_["wb"]], "wp%d" % it_["wb"]
                        hbuf, hres = hT[it_["hb"]], "hT%d" % it_["hb"]
                        ka_tok = start if start < OWN else (1024 if start == 1024 else 1280)
                        fm_chunks = {"FM": 4, "KAVA": 2, "TM": 0}[kind]
                        if fm_chunks:
                            sob = so[cst["so"] % 2]
                            sores = "so%d" % (cst["so"] % 2)
                            cst["so"] += 1
                            kqs = []

                            def mm_chunk(cc):
                                kq = cst["fm"] % 2
                                cst["fm"] += 1
                                kqs.append(kq)
                                for kc in range(16):
                                    mm(banks[kq][:, 0:ntok], wbuf[:, kc, cc * 128:(cc + 1) * 128], hbuf[:, kc, 0:ntok], kc == 0, kc == 15,
                                       [wres, hres], [PS[kq]])

                            def post_chunk(cc):
                                kq = kqs[cc]
                                pq, psw = banks[kq], banks[2 + kq]
                                if latent:
                                    xbb, t1b, t2b = xb16[kq], t1[kq], t2[kq]
                                    S.op("act", lambda e, xbb=xbb, pq=pq, ntok=ntok: e.activation(out=xbb[:, 0:ntok], in_=pq[:, 0:ntok], func=AF.Copy),
                                         reads=[PS[kq]], writes=["xb16_%d" % kq])
                                    mm(psw[:, 0:ntok], pswap[:], xbb[:, 0:ntok], True, True, ["pswap", "xb16_%d" % kq], [PS[2 + kq]])
                                    S.op("dve", lambda e, t1b=t1b, pq=pq, ntok=ntok, start=start: e.tensor_tensor(
                                        out=t1b[:, 0:ntok], in0=pq[:, 0:ntok], in1=ropeC[:, start:start + ntok], op=ALU.mult),
                                        reads=[PS[kq], "ropeC"], writes=["t1_%d" % kq])
                                    S.op("dve", lambda e, t2b=t2b, psw=psw, ntok=ntok, start=start: e.tensor_tensor(
                                        out=t2b[:, 0:ntok], in0=psw[:, 0:ntok], in1=ropeS[:, start:start + ntok], op=ALU.mult),
                                        reads=[PS[2 + kq], "ropeS"], writes=["t2_%d" % kq])
                                    S.op("dve", lambda e, sob=sob, cc=cc, t1b=t1b, t2b=t2b, ntok=ntok: e.tensor_tensor(
                                        out=sob[:, cc, 0:ntok], in0=t1b[:, 0:ntok], in1=t2b[:, 0:ntok], op=ALU.add),
                                        reads=["t1_%d" % kq, "t2_%d" % kq], writes=[(sores, cc)])
                                else:
                                    S.op("act", lambda e, sob=sob, cc=cc, pq=pq, ntok=ntok: e.activation(out=sob[:, cc, 0:ntok], in_=pq[:, 0:ntok], func=AF.Copy),
                                         reads=[PS[kq]], writes=[(sores, cc)])

                            mm_chunk(0)
                            for cc in range(fm_chunks):
                                if cc + 1 < fm_chunks:
                                    mm_chunk(cc + 1)
                                post_chunk(cc)
                            if kind == "FM":
                                dst = dest[:, slot0:slot0 + 4, start:start + ntok]
                            else:
                                dst = KTA_d[:, 0:2, ka_tok:ka_tok + ntok]
                            nm = uq("fm_out")
                            stores[pn].append(nm)
                            S.dma("sp", dst, sob[:, 0:fm_chunks, 0:ntok], reads=[(sores, c_) for c_ in range(fm_chunks)],
                                  writes=[nm])
                        if kind in ("TM", "KAVA"):
                            c0, ncols = (0, 512) if kind == "TM" else (256, 256)
                            for ts in range(ntok // 128):
                                kv_ = cst["tm"] % 2
                                cst["tm"] += 1
                                pv = banks[4 + kv_]
                                svb = sv[kv_]
                                for kc in range(16):
                                    mm(pv[:, 0:ncols], hbuf[:, kc, ts * 128:(ts + 1) * 128], wbuf[:, kc, c0:c0 + ncols], kc == 0, kc == 15,
                                       [wres, hres], [PS[4 + kv_]])
                                if kv_ == 0:
                                    S.op("act", lambda e, svb=svb, pv=pv, ncols=ncols: e.activation(out=svb[:, 0:ncols], in_=pv[:, 0:ncols], func=AF.Copy),
                                         reads=[PS[4]], writes=["sv0"])
                                else:
                                    S.op("dve", lambda e, svb=svb, pv=pv, ncols=ncols: e.tensor_copy(out=svb[:, 0:ncols], in_=pv[:, 0:ncols]),
                                         reads=[PS[5]], writes=["sv1"])
                                if kind == "TM":
                                    r0 = start + ts * 128
                                    dst = VB_d[r0:r0 + 128, slot0:slot0 + 512]
                                else:
                                    r0 = ka_tok + ts * 128
                                    dst = VA_d[r0:r0 + 128, 0:256]
                                nm = uq("tm_out")
                                stores[pn].append(nm)
                                S.dma("sp", dst, svb[:, 0:ncols], reads=["sv%d" % kv_], writes=[nm])

                    load_item(0)
                    for i in range(len(items)):
                        if i + 1 < len(items):
                            load_item(i + 1)
                        if items[i]["pn"] == 8 and items[i]["load_w"]:
                            S.dma("sp", KT0[:], KTB_d[:, 0:2, :], reads=stores[5], writes=["KT0_pre"])
                            for half in range(2):
                                S.dma("sp", V0[:, half * 17:(half + 1) * 17, 0:256],
                                      VB_d[half * 2176:(half + 1) * 2176, 0:256].rearrange("(kt p) c -> p kt c", p=128),
                                      reads=stores[7], writes=["V0_pre%d" % half])
                            S.dma("sp", QT0[:], QTB_d[:, 0:2, :], reads=stores[3], writes=["QT0_pre"])
                        compute_item(i)
                    S.flush()

            with contextlib.ExitStack() as ph:
                KT = [KT0, sbt(ph, "KT1", [128, 2, NTOK], BF16)]
                V = [V0, sbt(ph, "V1", [128, 34, 257], BF16)]
                QT = [QT0, sbt(ph, "QT1", [128, 2, OWN], BF16)]
                NE = 4
                E = [sbt(ph, "E%d" % i, [128, 512], BF16) for i in range(NE)]
                n1 = sbt(ph, "n1", [128, 4, 256], F32)
                dd = [sbt(ph, "dd%d" % i, [128, 256], F32) for i in range(4)]
                sq4 = [sbt(ph, "sq%d" % i, [128, 256], F32) for i in range(4)]
                ss = [sbt(ph, "ss%d" % i, [128, 4], F32) for i in range(4)]
                rz = [sbt(ph, "rz%d" % i, [128, 1], F32) for i in range(4)]
                gsub = sbt(ph, "gsub", [128, 256], F32)
                lamb = sbt(ph, "lamb", [128, 512], F32)
                lt = sbt(ph, "lt", [128, 8], F32)
                ob = [sbt(ph, "ob%d" % i, [128, 256], BF16) for i in range(4)]
                oT = [sbt(ph, "oT%d" % i, [128, 2, 128], BF16) for i in range(4)]
                wpan = [sbt(ph, "wpanB%d" % i, [128, 16, 512], BF16) for i in range(2)]
                pfm3 = banks[6][:, 256:448].rearrange("p (c j) -> p c j", j=2)
                neglam = lt[:, 6:7]
                for i in range(2):
                    S.op("dve", lambda e, i=i: e.memset(V[i][:, :, 256:257], 1.0), writes=[("V1", i)])
                S.dma("sp", gsub[:], subg_in.partition_broadcast(128), writes=["gsub"])
                S.op("dve", lambda e: e.tensor_scalar_mul(out=gsub[:], in0=gsub[:], scalar1=1.0 - LAM_INIT), reads=["gsub"], writes=["gsub"])
                S.dma("sp", lamb[:], lamv_in.partition_broadcast(128), writes=["lamb"])
                for i in range(2):
                    S.op("dve", lambda e, i=i: e.tensor_tensor(out=lamb[:, i * 256:i * 256 + 128], in0=lamb[:, i * 256:i * 256 + 128],
                                                             in1=lamb[:, i * 256 + 128:i * 256 + 256], op=ALU.mult),
                         reads=["lamb"], writes=["lamb"])
                    S.op("dve", lambda e, i=i: e.reduce_sum(out=lt[:, i:i + 1], in_=lamb[:, i * 256:i * 256 + 128], axis=AX.X),
                         reads=["lamb"], writes=["lt"])
                S.op("act", lambda e: e.activation(out=lt[:, 2:4], in_=lt[:, 0:2], func=AF.Exp), reads=["lt"], writes=["lt"])
                S.op("dve", lambda e: e.tensor_tensor(out=lt[:, 5:6], in0=lt[:, 3:4], in1=lt[:, 2:3], op=ALU.subtract), reads=["lt"], writes=["lt"])
                S.op("dve", lambda e: e.tensor_scalar_add(out=lt[:, 6:7], in0=lt[:, 5:6], scalar1=-LAM_INIT), reads=["lt"], writes=["lt"])

                def load_head(h):
                    hb_ = h % 2
                    S.dma("sp", KT[hb_][:], KTB_d[:, 2 * h:2 * h + 2, :], writes=[("KT", hb_)])
                    for half in range(2):
                        S.dma("sp", V[hb_][:, half * 17:(half + 1) * 17, 0:256],
                              VB_d[half * 2176:(half + 1) * 2176, h * 256:(h + 1) * 256].rearrange("(kt p) c -> p kt c", p=128),
                              writes=[("V", hb_, half)])
                    S.dma("sp", QT[hb_][:], QTB_d[:, 2 * h:2 * h + 2, :], writes=[("QT", hb_)])

                steps = [(h, qt, r, kt) for h in range(4) for qt in range(2) for r in range(2) for kt in range(34)]
                SB = (4, 5, 7)

                def emit_S(i):
                    h, qt, r, kt = steps[i]
                    hb_ = h % 2
                    ks = SB[i % 3]
                    mm(banks[ks][:], KT[hb_][:, r, kt * 128:(kt + 1) * 128], QT[hb_][:, r, qt * 512:(qt + 1) * 512], True, True,
                       [("KT", hb_), ("QT", hb_)], [PS[ks]])

                cntO = 0
                pending = []
                staged = []

                ada_q = []
                accs = [sbt(ph, "acc%d" % i, [128, 512], F32) for i in range(2)]

                def ada_gemv(jp):
                    buf, wres = wpan[jp % 2], "wpanB%d" % (jp % 2)
                    acc, ares = accs[jp % 2], "acc%d" % (jp % 2)
                    S.op("dve", lambda e: e.tensor_scalar(out=acc[:], in0=buf[:, 0, :], scalar1=cs_s[:, 0, 0:1], scalar2=None, op0=ALU.mult),
                         reads=[wres, "cs_s"], writes=[ares])
                    for kc in range(1, 16):
                        S.op("dve", lambda e, kc=kc: e.scalar_tensor_tensor(out=acc[:], in0=buf[:, kc, :], scalar=cs_s[:, kc, 0:1], in1=acc[:],
                                                                          op0=ALU.mult, op1=ALU.add),
                             reads=[wres, "cs_s", ares], writes=[ares])

                def ada_reduce(jp):
                    acc, ares = accs[jp % 2], "acc%d" % (jp % 2)
                    for cc in range(4):
                        mm(pfm3[:, jp * 4 + cc, :], acc[:, cc * 128:(cc + 1) * 128], ones_f[:, 0:2], True, True, [ares, "ones_f"], [PS[6]])

                def run_stage(k):
                    if not staged:
                        return
                    g = staged[0]
                    for qs, o_ in g["chain"]:
                        ddb, obb, ssb = dd[o_], ob[o_], ss[o_]
                        if k == 0:
                            S.op("dve", lambda e, qs=qs, ddb=ddb: e.tensor_tensor(out=ddb[:], in0=ddb[:], in1=n1[:, qs, :], op=ALU.add),
                                 reads=["dd%d" % o_, ("n1", qs)], writes=["dd%d" % o_])
                            S.op("dve", lambda e, ddb=ddb, sqb=sq4[qs]: e.tensor_tensor(out=sqb[:], in0=ddb[:], in1=ddb[:], op=ALU.mult),
                                 reads=["dd%d" % o_], writes=["sq%d" % qs])
                            S.op("dve", lambda e, ssb=ssb, sqb=sq4[qs]: e.reduce_sum(out=ssb[:, 0:1], in_=sqb[:], axis=AX.X),
                                 reads=["sq%d" % qs], writes=["ss%d" % o_])
                        elif k == 1:
                            S.op("act", lambda e, ssb=ssb: e.activation(out=ssb[:, 1:2], in_=ssb[:, 0:1], func=AF.Ln,
                                                                      bias=eps_sub[:, 0:1], scale=1.0 / 256.0),
                                 reads=["ss%d" % o_, "eps_sub"], writes=["ss%d" % o_])
                            S.op("act", lambda e, ssb=ssb: e.activation(out=ssb[:, 2:3], in_=ssb[:, 1:2], func=AF.Exp, scale=-0.5),
                                 reads=["ss%d" % o_], writes=["ss%d" % o_])
                        elif k == 2:
                            S.op("dve", lambda e, ssb=ssb, ddb=ddb, obb=obb: e.scalar_tensor_tensor(
                                out=obb[:], in0=ddb[:], scalar=ssb[:, 2:3], in1=gsub[:], op0=ALU.mult, op1=ALU.mult),
                                reads=["dd%d" % o_, "ss%d" % o_, "gsub"], writes=["ob%d" % o_])
                            pending.append((o_, g["h"], g["qt"] * 512 + qs * 128))
                    if k == 2:
                        staged.pop(0)

                def flush_pending():
                    while pending:
                        o_, h_, tok0 = pending.pop(0)
                        obb, oTb = ob[o_], oT[o_]
                        pT = banks[6][:].bitcast(BF16)[:, (o_ % 2) * 256:(o_ % 2 + 1) * 256].rearrange("p (c t) -> p c t", t=128)
                        for c_ in range(2):
                            S.op("pe", lambda e, pT=pT, c_=c_, obb=obb: e.transpose(pT[:, c_, :], obb[:, c_ * 128:(c_ + 1) * 128], ident_b[:]),
                                 reads=["ob%d" % o_, "ident_b"], writes=[PS[6]])
                        S.op("act", lambda e, pT=pT, oTb=oTb: e.activation(out=oTb[:], in_=pT, func=AF.Copy),
                             reads=[PS[6]], writes=["oT%d" % o_])
                        S.dma("sp", AOT_d[8 + 2 * h_:10 + 2 * h_, :, tok0:tok0 + 128].rearrange("c p t -> p c t"), oTb[:],
                              reads=["oT%d" % o_], writes=[uq("AOT_d")])

                emit_S(0)
                emit_S(1)
                for i, (h, qt, r, kt) in enumerate(steps):
                    hb_ = h % 2
                    Vb = V[hb_]
                    if kt == 20 and ada_q:
                        ada_gemv(ada_q[0])
                    if kt == 30 and ada_q:
                        ada_reduce(ada_q.pop(0))
                    if kt == 3:
                        run_stage(0)
                    elif kt == 7:
                        run_stage(1)
                    elif kt == 11:
                        run_stage(2)
                    elif kt == 15:
                        flush_pending()
                    if qt == 0 and r == 0 and kt == 0 and h + 1 < 4:
                        load_head(h + 1)
                    if i + 2 < len(steps):
                        emit_S(i + 2)
                    ks = SB[i % 3]
                    Eb = E[i % NE]
                    eres = "E%d" % (i % NE)
                    S.op("act", lambda e, Eb=Eb, ks=ks: e.activation(out=Eb[:], in_=banks[ks][:], func=AF.Exp, scale=SCALE),
                         reads=[PS[ks]], writes=[eres])
                    for qs in range(4):
                        mm(banks[qs][:, 0:257], Eb[:, qs * 128:(qs + 1) * 128], Vb[:, kt, :], kt == 0, kt == 33,
                           [eres, ("V", hb_, kt // 17), ("V1", hb_)], [PS[qs]])
                    if kt != 33:
                        continue
                    chain = []
                    for qs in range(4):
                        S.op("dve", lambda e, qs=qs: e.reciprocal(out=rz[qs][:], in_=banks[qs][:, 256:257]),
                             reads=[PS[qs]], writes=["rz%d" % qs])
                        if r == 0:
                            S.op("dve", lambda e, qs=qs: e.tensor_scalar(out=n1[:, qs, :], in0=banks[qs][:, 0:256], scalar1=rz[qs][:, 0:1],
                                                                      scalar2=None, op0=ALU.mult),
                                 reads=[PS[qs], "rz%d" % qs], writes=[("n1", qs)])
                        else:
                            o_ = cntO % 4
                            cntO += 1
                            ddb = dd[o_]
                            S.op("dve", lambda e, qs=qs, ddb=ddb: e.tensor_scalar(out=ddb[:], in0=banks[qs][:, 0:256], scalar1=rz[qs][:, 0:1],
                                                                                 scalar2=neglam, op0=ALU.mult, op1=ALU.mult),
                                 reads=[PS[qs], "rz%d" % qs, "lt"], writes=["dd%d" % o_])
                            chain.append((qs, o_))
                    if chain:
                        staged.append(dict(chain=list(chain), h=h, qt=qt))
                    jp = 8 + (h * 4 + qt * 2 + r)
                    S.dma("pool", wpan[jp % 2][:], w_ada_p[jp], writes=["wpanB%d" % (jp % 2)])
                    ada_q.append(jp)
                while ada_q:
                    ada_gemv(ada_q[0])
                    ada_reduce(ada_q.pop(0))
                for k_ in range(3):
                    run_stage(k_)
                flush_pending()
                S.flush()

        with contextlib.ExitStack() as ph56:
            h2Tb = sbt(ph56, "h2Tb", [128, 16, OWN], BF16)

            def layer_norm(src, dst, gtile, btile, stt, tagres):
                (sap, sres), (dap, dres) = src, dst
                sres = list(sres) if isinstance(sres, list) else [sres]
                st6, mv = stt
                for c in range(4):
                    S.op("dve", lambda e, c=c: e.bn_stats(out=st6[:, c, :], in_=sap[:, c * 512:(c + 1) * 512]),
                         reads=sres, writes=[tagres + "st"])
                S.op("dve", lambda e: e.bn_aggr(out=mv[:, 0:2], in_=st6[:]), reads=[tagres + "st"], writes=[tagres + "mv"])
                S.op("act", lambda e: e.activation(out=mv[:, 2:3], in_=mv[:, 1:2], func=AF.Ln, bias=eps_ln[:, 0:1], scale=1.0),
                     reads=[tagres + "mv", "eps_ln"], writes=[tagres + "mv"])
                S.op("act", lambda e: e.activation(out=mv[:, 3:4], in_=mv[:, 2:3], func=AF.Exp, scale=-0.5),
                     reads=[tagres + "mv"], writes=[tagres + "mv"])
                S.op("dve", lambda e: e.scalar_tensor_tensor(out=dap, in0=sap, scalar=mv[:, 0:1], in1=gtile[0], op0=ALU.subtract, op1=ALU.mult),
                     reads=sres + [tagres + "mv", gtile[1]], writes=[dres])
                S.op("dve", lambda e: e.scalar_tensor_tensor(out=dap, in0=dap, scalar=mv[:, 3:4], in1=btile[0], op0=ALU.mult, op1=ALU.add),
                     reads=[dres, tagres + "mv", btile[1]], writes=[dres])

            with contextlib.ExitStack() as phwo:
                wo = sbt(phwo, "wo", [128, 16, D], BF16)
                for q4 in range(4):
                    S.dma("pool", wo[:, q4 * 4:(q4 + 1) * 4, :], w_out_p[:, q4 * 4:(q4 + 1) * 4, :], writes=[("wo", q4)])
                with contextlib.ExitStack() as ph:
                    QTA = sbt(ph, "QTA", [128, 8, OWN], BF16)
                    KTA = sbt(ph, "KTA", [128, 2, 1536], BF16)
                    VA = sbt(ph, "VA", [128, 12, 256], BF16)
                    sinkb = sbt(ph, "sinkb", [128, 8], F32)
                    sexp = sbt(ph, "sexp", [128, 8], F32)
                    sinkexp = sbt(ph, "sinkexp", [128, 8, 128], F32)
                    EA = [sbt(ph, "EA%d" % i, [128, 4, 128], BF16) for i in range(3)]
                    den = sbt(ph, "den", [128, 512], F32)
                    oa = [sbt(ph, "oa%d" % i, [128, 4, 128], BF16) for i in range(2)]
                    S.dma("sp", QTA[:], QTA_d, writes=["QTA"])
                    S.dma("sp", KTA[:], KTA_d, writes=["KTA"])
                    S.dma("sp", VA[:], VA_d.rearrange("(kt p) c -> p kt c", p=128), writes=["VA"])
                    S.dma("sp", sinkb[:], sink_in.partition_broadcast(128), writes=["sinkb"])
                    S.op("act", lambda e: e.activation(out=sexp[:], in_=sinkb[:], func=AF.Exp), reads=["sinkb"], writes=["sexp"])
                    S.op("dve", lambda e: e.tensor_copy(out=sinkexp[:], in_=sexp[:].unsqueeze(2).to_broadcast([128, 8, 128])),
                         reads=["sexp"], writes=["sinkexp"])
                    dg = [sbt(ph, "dg%d" % i, [128, 4, 128], F32) for i in range(2)]
                    gb = sbt(ph, "gb", [128, D], F32)
                    pfm3 = banks[6][:, 256:448].rearrange("p (c j) -> p c j", j=2)
                    S.op("dve", lambda e: e.tensor_tensor(
                        out=modFM[:, 32:96, :], in0=pfm3[:, 32:96, :],
                        in1=bfm[:, 32:96].unsqueeze(2).to_broadcast([128, 64, 2]), op=ALU.add),
                        reads=[PS[6], "bfm"], writes=["modFM"])
                    S.op("dve", lambda e: e.tensor_scalar_add(out=modFM[:, 64:80, :], in0=modFM[:, 64:80, :], scalar1=1.0),
                         reads=["modFM"], writes=["modFM"])
                    for gi, base in ((0, 32), (1, 80)):
                        for q4 in range(4):
                            dgb = dg[q4 % 2]
                            dres = "dg%d" % (q4 % 2)
                            S.op("dve", lambda e, dgb=dgb, base=base, q4=q4: e.tensor_tensor(
                                out=dgb[:], in0=ident_f[:].unsqueeze(1).to_broadcast([128, 4, 128]),
                                in1=modFM[:, base + 4 * q4:base + 4 * q4 + 4, 0:1].to_broadcast([128, 4, 128]), op=ALU.mult),
                                reads=["ident_f", "modFM"], writes=[dres])
                            mm(banks[q4][:], ones_f[:], dgb[:].rearrange("p a b -> p (a b)"), True, True, ["ones_f", dres], [PS[q4]])
                        for c in range(4):
                            S.op("act", lambda e, c=c: e.activation(out=gb[:, c * 512:(c + 1) * 512], in_=banks[c][:], func=AF.Copy),
                                 reads=[PS[c]], writes=[("gb", c)])
                        S.dma("sp", G_d[gi], gb[:], reads=[("gb", c) for c in range(4)], writes=[uq("G_d")])
                    NEA = 4
                    EA4 = EA + [sbt(ph, "EA3", [128, 4, 128], BF16)]
                    steps = []
                    for n in range(8):
                        for kv in range(2):
                            keys = [((n - 1) if n > 0 else 8, 0 if n > 0 else 2), (n, None),
                                    ((n + 1) if n < 7 else 9, 1 if n < 7 else 3), (10, None), (11, None)]
                            for j, (ktile, m) in enumerate(keys):
                                steps.append((n, kv, j, ktile, m))
                    SB = (4, 5, 6)

                    def emit_SA(i):
                        n, kv, j, ktile, m = steps[i]
                        ks = SB[i % 3]
                        mm(banks[ks][:], KTA[:, kv, ktile * 128:(ktile + 1) * 128], QTA[:, kv * 4:(kv + 1) * 4, n * 128:(n + 1) * 128], True, True,
                           ["KTA", "QTA"], [PS[ks]])

                    emit_SA(0)
                    emit_SA(1)
                    for i, (n, kv, j, ktile, m) in enumerate(steps):
                        it = n * 2 + kv
                        kO, kZ = (it % 2) * 2, (it % 2) * 2 + 1
                        oab = oa[it % 2]
                        oares = "oa%d" % (it % 2)
                        if i + 2 < len(steps):
                            emit_SA(i + 2)
                        ks = SB[i % 3]
                        Eb = EA4[i % NEA]
                        eres = "EA%d" % (i % NEA)
                        pS3 = banks[ks][:].rearrange("p (g t) -> p g t", t=128)
                        S.op("act", lambda e, Eb=Eb, pS3=pS3: e.activation(out=Eb[:], in_=pS3, func=AF.Exp, scale=SCALE),
                             reads=[PS[ks]], writes=[eres])
                        if m is not None:
                            S.op("dve", lambda e, Eb=Eb, m=m: e.tensor_tensor(out=Eb[:], in0=Eb[:], in1=masks[:, m:m + 1, :].to_broadcast([128, 4, 128]),
                                                                            op=ALU.mult),
                                 reads=[eres, "masks"], writes=[eres])
                        mm(banks[kO][:], VA[:, ktile, kv * 128:(kv + 1) * 128], Eb[:], j == 0, j == 4, ["VA", eres], [PS[kO]])
                        mm(banks[kZ][:], ones_b[:], Eb[:], j == 0, j == 4, ["ones_b", eres], [PS[kZ]])
                        if j != 4:
                            continue
                        S.op("dve", lambda e, kZ=kZ, kv=kv: e.tensor_tensor(out=den[:], in0=banks[kZ][:],
                                                                          in1=sinkexp[:, kv * 4:(kv + 1) * 4, :].rearrange("p g t -> p (g t)"), op=ALU.add),
                             reads=[PS[kZ], "sinkexp"], writes=["den"])
                        S.op("act", lambda e: e.activation(out=den[:], in_=den[:], func=AF.Ln), reads=["den"], writes=["den"])
                        S.op("act", lambda e: e.activation(out=den[:], in_=den[:], func=AF.Exp, scale=-1.0), reads=["den"], writes=["den"])
                        S.op("dve", lambda e, kO=kO, oab=oab: e.tensor_tensor(out=oab[:].rearrange("p g t -> p (g t)"), in0=banks[kO][:], in1=den[:], op=ALU.mult),
                             reads=[PS[kO], "den"], writes=[oares])
                        S.dma("sp", AOT_d[kv * 4:(kv + 1) * 4, :, n * 128:(n + 1) * 128].rearrange("c p t -> p c t"), oab[:],
                              reads=[oares], writes=[uq("AOT_dA")])
                    S.flush()

                with contextlib.ExitStack() as ph:
                    G1B = sbt(ph, "G1B", [128, D], F32)
                    l1g = sbt(ph, "l1g", [128, D], F32)
                    l1b = sbt(ph, "l1b", [128, D], F32)
                    wr = sbt(ph, "wr", [128, 16, 36], F32)
                    brb = sbt(ph, "brb", [128, 36], F32)
                    aot = [sbt(ph, "aot%d" % i, [128, 16, 128], BF16) for i in range(2)]
                    xt = [sbt(ph, "xt%d" % i, [128, D], F32) for i in range(2)]
                    rr = sbt(ph, "rr", [128, D], F32)
                    x1 = [sbt(ph, "x1_%d" % i, [128, D], F32) for i in range(2)]
                    h2Tf = sbt(ph, "h2Tf", [128, 16, 128], F32)
                    st6 = sbt(ph, "st6", [128, 4, 6], F32)
                    mv = sbt(ph, "mv", [128, 4], F32)
                    rt = sbt(ph, "rt", [128, 64], F32)
                    lg = sbt(ph, "lg", [128, 36], F32)
                    lm = sbt(ph, "lm", [128, 32], F32)
                    s1 = sbt(ph, "s1", [128, 32], F32)
                    s2 = sbt(ph, "s2", [128, 32], F32)
                    m8 = sbt(ph, "m8", [128, 8], F32)
                    wtT = sbt(ph, "wtT", [32, 128], F32)
                    S.dma("sp", G1B[:], G_d[0], writes=["G1B"])
                    S.dma("sp", l1g[:], ln1g_in.partition_broadcast(128), writes=["l1g"])
                    S.dma("sp", l1b[:], ln1b_in.partition_broadcast(128), writes=["l1b"])
                    S.dma("sp", wr[:], w_r_in, writes=["wr"])
                    S.dma("sp", brb[:], b_r_in.partition_broadcast(128), writes=["brb"])
                    def load5(t):
                        S.dma("sp", aot[t % 2][:], AOT_d[:, :, t * 128:(t + 1) * 128].rearrange("k p t -> p k t"), writes=["aot%d" % (t % 2)])
                        S.dma("sp", xt[t % 2][:], xs[t * 128:(t + 1) * 128, :], writes=["xt%d" % (t % 2)])

                    def stageA(t):
                        ab, xtb, x1b = aot[t % 2], xt[t % 2], x1[t % 2]
                        ares, xres, x1res = "aot%d" % (t % 2), "xt%d" % (t % 2), "x1_%d" % (t % 2)
                        for c in range(4):
                            for kc in range(16):
                                mm(banks[c][:], ab[:, kc, :], wo[:, kc, c * 512:(c + 1) * 512], kc == 0, kc == 15,
                                   [ares, ("wo", kc // 4)], [PS[c]])

                    def stageA2(t):
                        ab, xtb, x1b = aot[t % 2], xt[t % 2], x1[t % 2]
                        ares, xres, x1res = "aot%d" % (t % 2), "xt%d" % (t % 2), "x1_%d" % (t % 2)
                        for c in range(4):
                            S.op("dve", lambda e, c=c: e.tensor_tensor(out=rr[:, c * 512:(c + 1) * 512], in0=banks[c][:],
                                                                     in1=G1B[:, c * 512:(c + 1) * 512], op=ALU.mult),
                                 reads=[PS[c], "G1B"], writes=[("rr", c)])
                        S.op("dve", lambda e, xtb=xtb: e.scalar_tensor_tensor(out=rr[:], in0=xtb[:], scalar=ALPHA, in1=rr[:], op0=ALU.mult, op1=ALU.add),
                             reads=[xres] + [("rr", c) for c in range(4)], writes=[("rr", c) for c in range(4)])
                        layer_norm((rr[:], [("rr", c) for c in range(4)]), (x1b[:], x1res), (l1g[:], "l1g"), (l1b[:], "l1b"), (st6, mv), "ln1")
                        S.dma("sp", X1_d[t * 128:(t + 1) * 128, :], x1b[:], reads=[x1res], writes=[uq("X1_d")])
                    def stageBC(t):
                        x1b = x1[t % 2]
                        x1res = "x1_%d" % (t % 2)
                        for kc in range(16):
                            kb_ = 4 + (kc // 4) % 2
                            S.op("pe", lambda e, kc=kc, kb_=kb_, x1b=x1b: e.transpose(banks[kb_][:, (kc % 4) * 128:(kc % 4 + 1) * 128],
                                                                                   x1b[:, kc * 128:(kc + 1) * 128], ident_f[:]),
                                 reads=[x1res, "ident_f"], writes=[PS[kb_]])
                            if kc % 4 == 3:
                                for k2 in range(kc - 3, kc + 1):
                                    S.op("act", lambda e, k2=k2, kb_=kb_: e.activation(
                                        out=h2Tf[:, k2, :], in_=banks[kb_][:, (k2 % 4) * 128:(k2 % 4 + 1) * 128], func=AF.Identity,
                                        bias=modFM[:, 48 + k2, 0:1], scale=modFM[:, 64 + k2, 0:1]),
                                        reads=[PS[kb_], "modFM"], writes=[("h2Tf", k2 // 4)])
                        S.op("pool", lambda e, t=t: e.tensor_copy(out=h2Tb[:, :, t * 128:(t + 1) * 128], in_=h2Tf[:]),
                             reads=[("h2Tf", q_) for q_ in range(4)], writes=[("h2Tb", t)])
                        for kc in range(16):
                            mm(banks[6][:, 0:36], h2Tf[:, kc, :], wr[:, kc, :], kc == 0, kc == 15, [("h2Tf", kc // 4), "wr"], [PS[6]])

                    def stageBCchain(t):
                        R = "rt"
                        S.op("dve", lambda e: e.tensor_tensor(out=lg[:], in0=banks[6][:, 0:36], in1=brb[:], op=ALU.add), reads=[PS[6], "brb"], writes=["lg"])
                        S.op("dve", lambda e: e.reduce_max(out=rt[:, 0:1], in_=lg[:, 0:4], axis=AX.X), reads=["lg"], writes=[R])
                        S.op("dve", lambda e: e.tensor_scalar_mul(out=rt[:, 1:2], in0=rt[:, 0:1], scalar1=-1.0), reads=[R], writes=[R])
                        S.op("act", lambda e: e.activation(out=rt[:, 4:8], in_=lg[:, 0:4], func=AF.Exp, bias=rt[:, 1:2], scale=1.0), reads=["lg", R], writes=[R])
                        S.op("dve", lambda e: e.reduce_sum(out=rt[:, 2:3], in_=rt[:, 4:8], axis=AX.X), reads=[R], writes=[R])
                        S.op("dve", lambda e: e.reciprocal(out=rt[:, 3:4], in_=rt[:, 2:3]), reads=[R], writes=[R])
                        S.op("dve", lambda e: e.tensor_scalar(out=rt[:, 8:12], in0=lg[:, 0:4], scalar1=rt[:, 0:1], scalar2=None, op0=ALU.is_equal),
                             reads=["lg", R], writes=[R])
                        S.op("dve", lambda e: e.tensor_tensor(out=lm[:].rearrange("p (g j) -> p g j", j=8), in0=lg[:, 4:36].rearrange("p (g j) -> p g j", j=8),
                                                              in1=rt[:, 8:12].unsqueeze(2).to_broadcast([128, 4, 8]), op=ALU.mult),
                             reads=["lg", R], writes=["lm"])
                        S.op("dve", lambda e: e.tensor_reduce(out=rt[:, 16:24], in_=lm[:].rearrange("p (g j) -> p j g", j=8), axis=AX.X, op=ALU.add),
                             reads=["lm"], writes=[R])
                        S.op("dve", lambda e: e.max(out=m8[:], in_=rt[:, 16:24]), reads=[R], writes=["m8"])
                        S.op("dve", lambda e: e.tensor_tensor(out=rt[:, 24:25], in0=m8[:, 1:2], in1=m8[:, 0:1], op=ALU.subtract), reads=["m8"], writes=[R])
                        S.op("act", lambda e: e.activation(out=rt[:, 25:26], in_=rt[:, 24:25], func=AF.Exp), reads=[R], writes=[R])
                        S.op("dve", lambda e: e.tensor_scalar_add(out=rt[:, 26:27], in0=rt[:, 25:26], scalar1=1.0), reads=[R], writes=[R])
                        S.op("dve", lambda e: e.reciprocal(out=rt[:, 27:28], in_=rt[:, 26:27]), reads=[R], writes=[R])
                        S.op("dve", lambda e: e.tensor_tensor(out=rt[:, 28:29], in0=rt[:, 25:26], in1=rt[:, 27:28], op=ALU.mult), reads=[R], writes=[R])
                        S.op("dve", lambda e: e.tensor_scalar(out=rt[:, 29:31], in0=rt[:, 27:29], scalar1=rt[:, 3:4], scalar2=None, op0=ALU.mult),
                             reads=[R], writes=[R])
                        S.op("dve", lambda e: e.tensor_scalar(out=s1[:], in0=lm[:], scalar1=m8[:, 0:1], scalar2=rt[:, 29:30], op0=ALU.is_equal, op1=ALU.mult),
                             reads=["lm", "m8", R], writes=["s1"])
                        S.op("dve", lambda e: e.tensor_scalar(out=s2[:], in0=lm[:], scalar1=m8[:, 1:2], scalar2=rt[:, 30:31], op0=ALU.is_equal, op1=ALU.mult),
                             reads=["lm", "m8", R], writes=["s2"])
                        S.op("dve", lambda e: e.tensor_tensor(out=s1[:], in0=s1[:], in1=s2[:], op=ALU.add), reads=["s1", "s2"], writes=["s1"])
                        S.op("dve", lambda e: e.tensor_tensor(out=s1[:].rearrange("p (g j) -> p g j", j=8), in0=s1[:].rearrange("p (g j) -> p g j", j=8),
                                                              in1=rt[:, 8:12].unsqueeze(2).to_broadcast([128, 4, 8]), op=ALU.mult),
                             reads=["s1", R], writes=["s1"])
                    def stageBC2(t):
                        S.op("pe", lambda e: e.transpose(banks[7][0:32, 0:128], s1[:], ident_f[:]), reads=["s1", "ident_f"], writes=[PS[7]])
                        S.op("act", lambda e: e.activation(out=wtT[:], in_=banks[7][0:32, 0:128], func=AF.Copy), reads=[PS[7]], writes=["wtT"])
                        S.dma("sp", Wt_d[:, t * 128:(t + 1) * 128], wtT[:], reads=["wtT"], writes=[uq("Wt_d")])
                    load5(0)
                    load5(1)
                    stageA(0)
                    stageA2(0)
                    stageA(1)
                    for t in range(8):
                        if t + 2 < 8:
                            load5(t + 2)
                        stageBC(t)
                        if t + 1 < 8:
                            stageA2(t + 1)
                        stageBCchain(t)
                        if t + 2 < 8:
                            stageA(t + 2)
                        stageBC2(t)
                    S.flush()

            with contextlib.ExitStack() as ph67:
                yacc = sbt(ph67, "yacc", [128, 8, D], F32)
                with contextlib.ExitStack() as ph:
                    ring = [sbt(ph, "ring%d" % i, [128, 8192], BF16) for i in range(4)]
                    wtb = [sbt(ph, "wtb%d" % i, [128, OWN], F32) for i in range(2)]
                    sg = [sbt(ph, "sg%d" % i, [128, 512], F32) for i in range(2)]
                    tm = [sbt(ph, "tm%d" % i, [128, 512], F32) for i in range(2)]
                    actT = [sbt(ph, "actT%d" % i, [128, 4, 512], BF16) for i in range(2)]
                    cntR = 0
                    cntD = 0
                    for ei in range(n_exp):
                        slots = []
                        for wi, src in enumerate((wg_p, wu_p, wd_p)):
                            k = cntR % 4
                            cntR += 1
                            S.dma("pool", ring[k][:], src[ei].rearrange("p a b -> p (a b)"), writes=["ring%d" % k])
                            slots.append(k)
                        Gt = ring[slots[0]][:].rearrange("p (a b) -> p a b", b=512)
                        Ut = ring[slots[1]][:].rearrange("p (a b) -> p a b", b=512)
                        Dt = ring[slots[2]][:].rearrange("p (a b) -> p a b", b=D)
                        gres, ures, dres = ["ring%d" % k for k in slots]
                        wb = wtb[ei % 2]
                        wbres = "wtb%d" % (ei % 2)
                        S.dma("sp", wb[:], Wt_d[ei:ei + 1, :].partition_broadcast(128), writes=[wbres])
                        for tt in range(2):
                            for c in range(4):
                                kg, ku = c % 2, 2 + c % 2
                                for kc in range(16):
                                    mm(banks[kg][:], Gt[:, kc, c * 128:(c + 1) * 128], h2Tb[:, kc, tt * 512:(tt + 1) * 512], kc == 0, kc == 15,
                                       [gres, "h2Tb"], [PS[kg]])
                                for kc in range(16):
                                    mm(banks[ku][:], Ut[:, kc, c * 128:(c + 1) * 128], h2Tb[:, kc, tt * 512:(tt + 1) * 512], kc == 0, kc == 15,
                                       [ures, "h2Tb"], [PS[ku]])
                                sgb, tmb = sg[c % 2], tm[c % 2]
                                S.op("act", lambda e, sgb=sgb, kg=kg: e.activation(out=sgb[:], in_=banks[kg][:], func=AF.Silu),
                                     reads=[PS[kg]], writes=["sg%d" % (c % 2)])
                                S.op("dve", lambda e, sgb=sgb, tmb=tmb, ku=ku: e.tensor_tensor(out=tmb[:], in0=sgb[:], in1=banks[ku][:], op=ALU.mult),
                                     reads=["sg%d" % (c % 2), PS[ku]], writes=["tm%d" % (c % 2)])
                                S.op("dve", lambda e, tmb=tmb, tt=tt, c=c, wb=wb: e.tensor_tensor(out=actT[tt][:, c, :], in0=tmb[:],
                                                                                           in1=wb[:, tt * 512:(tt + 1) * 512], op=ALU.mult),
                                     reads=["tm%d" % (c % 2), wbres], writes=[("actT", tt, c)])
                        for tt in range(2):
                            for ts in range(4):
                                tile = tt * 4 + ts
                                for n4 in range(4):
                                    kd = 4 + cntD % 4
                                    cntD += 1
                                    for c in range(4):
                                        mm(banks[kd][:], actT[tt][:, c, ts * 128:(ts + 1) * 128], Dt[:, c, n4 * 512:(n4 + 1) * 512], c == 0, c == 3,
                                           [("actT", tt, c), dres], [PS[kd]])
                                    yres = ("yacc", tile, n4)
                                    if ei == 0:
                                        S.op("act", lambda e, kd=kd, tile=tile, n4=n4: e.activation(out=yacc[:, tile, n4 * 512:(n4 + 1) * 512],
                                                                                               in_=banks[kd][:], func=AF.Copy),
                                             reads=[PS[kd]], writes=[yres])
                                    else:
                                        S.op("dve", lambda e, kd=kd, tile=tile, n4=n4: e.tensor_tensor(
                                            out=yacc[:, tile, n4 * 512:(n4 + 1) * 512], in0=banks[kd][:],
                                            in1=yacc[:, tile, n4 * 512:(n4 + 1) * 512], op=ALU.add),
                                            reads=[PS[kd], yres], writes=[yres])
                    S.flush()

                with contextlib.ExitStack() as ph:
                    G2B = sbt(ph, "G2B", [128, D], F32)
                    l2g = sbt(ph, "l2g", [128, D], F32)
                    l2b = sbt(ph, "l2b", [128, D], F32)
                    x1t = [sbt(ph, "x1t%d" % i, [128, D], F32) for i in range(2)]
                    ot = [sbt(ph, "ot%d" % i, [128, D], F32) for i in range(2)]
                    st6b = sbt(ph, "st6b", [128, 4, 6], F32)
                    mvb = sbt(ph, "mvb", [128, 4], F32)
                    S.dma("sp", G2B[:], G_d[1], writes=["G2B"])
                    S.dma("sp", l2g[:], ln2g_in.partition_broadcast(128), writes=["l2g"])
                    S.dma("sp", l2b[:], ln2b_in.partition_broadcast(128), writes=["l2b"])
                    outs = []
                    S.dma("sp", x1t[0][:], X1_d[0:128, :], writes=["x1t0"])
                    for t in range(8):
                        xb_, ob_ = x1t[t % 2], ot[t % 2]
                        xres, ores = "x1t%d" % (t % 2), "ot%d" % (t % 2)
                        if t + 1 < 8:
                            S.dma("sp", x1t[(t + 1) % 2][:], X1_d[(t + 1) * 128:(t + 2) * 128, :], writes=["x1t%d" % ((t + 1) % 2)])
                        yv = yacc[:, t, :]
                        S.op("dve", lambda e, yv=yv: e.tensor_tensor(out=yv, in0=yv, in1=G2B[:], op=ALU.mult), reads=["G2B"], writes=[("y", t)])
                        S.op("dve", lambda e, yv=yv, xb_=xb_: e.scalar_tensor_tensor(out=yv, in0=xb_[:], scalar=ALPHA, in1=yv, op0=ALU.mult, op1=ALU.add),
                             reads=[xres, ("y", t)], writes=[("y", t)])
                        layer_norm((yv, ("y", t)), (ob_[:], ores), (l2g[:], "l2g"), (l2b[:], "l2b"), (st6b, mvb), "ln2")
                        S.dma("sp", out[t * 128:(t + 1) * 128, :], ob_[:], reads=[ores], writes=[("out", t)])
                        outs.append(("out", t))
                    S.op("sp", None, reads=outs)
                    S.flush()
    return nc


def _core_perm(q):
    own = list(range(q * 8, q * 8 + 8))
    prev = q * 8 - 1 if q > 0 else 31
    nxt = q * 8 + 8 if q < 3 else 0
    rest = [b for b in range(32) if b not in own and b != prev and b != nxt]
    blocks = own + [prev, nxt] + rest
    perm = np.concatenate([np.arange(b * 128, (b + 1) * 128) for b in blocks])
    return perm


def _rope_tables(perm):
    row = (perm // 64).astype(np.float32)
    col = (perm % 64).astype(np.float32)
    inv_freq = (10000.0 ** (-np.arange(32, dtype=np.float32) / np.float32(32))).astype(np.float32)
    ang_r = (row[None, :] * inv_freq[:, None]).astype(np.float32)
    ang_c = (col[None, :] * inv_freq[:, None]).astype(np.float32)
    C = np.concatenate([np.cos(ang_r), np.cos(ang_r), np.cos(ang_c), np.cos(ang_c)], axis=0).astype(np.float32)
    Sg = np.concatenate([-np.sin(ang_r), np.sin(ang_r), -np.sin(ang_c), np.sin(ang_c)], axis=0).astype(np.float32)
    return np.ascontiguousarray(C), np.ascontiguousarray(Sg)


def _prep_shared(inp):
    f = lambda a: np.ascontiguousarray(np.asarray(a, dtype=np.float32))
    sh = {}
    w_ada = f(inp["w_ada"])[0]
    sh["w_ada_p"] = np.ascontiguousarray(w_ada.reshape(16, 128, 24, 512).transpose(2, 1, 0, 3))
    b_ada = f(inp["b_ada"])[0]
    sh["b_ada_fm"] = np.ascontiguousarray(b_ada.reshape(96, 128).T)
    sh["b_ada_row"] = np.ascontiguousarray(b_ada.reshape(1, -1))
    w_in = f(inp["w_in"])[0]
    sh["w_in_p"] = np.ascontiguousarray(w_in.reshape(16, 128, 9, 512).transpose(2, 1, 0, 3))
    w_out = f(inp["w_out"])[0]
    sh["w_out_p"] = np.ascontiguousarray(w_out.reshape(16, 128, D).transpose(1, 0, 2))
    w_r = np.concatenate([f(inp["w_router_group"])[0], f(inp["w_router_expert"])[0]], axis=1)
    sh["w_r"] = np.ascontiguousarray(w_r.reshape(16, 128, 36).transpose(1, 0, 2))
    sh["b_r"] = np.concatenate([f(inp["b_router_group"])[0], f(inp["b_router_expert"])[0]]).reshape(1, 36)
    sh["wg_p"] = np.ascontiguousarray(f(inp["w_gate"])[0].reshape(NEXP, 16, 128, 512).transpose(0, 2, 1, 3))
    sh["wu_p"] = np.ascontiguousarray(f(inp["w_up"])[0].reshape(NEXP, 16, 128, 512).transpose(0, 2, 1, 3))
    sh["wd_p"] = np.ascontiguousarray(f(inp["w_down"])[0].reshape(NEXP, 4, 128, D).transpose(0, 2, 1, 3))
    sh["sink"] = f(inp["sink"]).reshape(1, 8)
    sh["lamv"] = np.concatenate([f(inp[k])[0] for k in ("lam_q1", "lam_k1", "lam_q2", "lam_k2")]).reshape(1, 512)
    sh["subln_g"] = f(inp["subln_g"]).reshape(1, 256)
    for k in ("ln1_g", "ln1_b", "ln2_g", "ln2_b"):
        sh[k] = f(inp[k]).reshape(1, D)
    sh["ident_b"] = np.eye(128, dtype=np.float32).astype(ml_dtypes.bfloat16)
    sh["ident_f"] = np.eye(128, dtype=np.float32)
    partner = np.concatenate([np.arange(32, 64), np.arange(0, 32), np.arange(96, 128), np.arange(64, 96)])
    psw = np.zeros((128, 128), np.float32)
    psw[partner, np.arange(128)] = 1.0
    sh["pswap"] = psw.astype(ml_dtypes.bfloat16)
    return sh


def _prep_core(inp, sh, core):
    b, q = core // 4, core % 4
    perm = _core_perm(q)
    m = dict(sh)
    x = np.asarray(inp["x"], dtype=np.float32)
    m["xs"] = np.ascontiguousarray(x[b][perm])
    m["ctxs"] = np.ascontiguousarray(np.asarray(inp["ctx"], dtype=np.float32)[b])
    cvec = np.stack([np.asarray(inp["c"], dtype=np.float32)[b], np.asarray(inp["c_ctx"], dtype=np.float32)], axis=1)
    m["cs"] = np.ascontiguousarray(cvec.reshape(16, 128, 2).transpose(1, 0, 2))
    C, Sg = _rope_tables(perm)
    m["ropeC"], m["ropeS"] = C, Sg
    jj = np.arange(128)[:, None]
    ii = np.arange(128)[None, :]
    tri_prev = (jj >= ii).astype(np.float32)
    tri_next = (jj <= ii).astype(np.float32)
    z = np.zeros((128, 128), np.float32)
    masks = np.stack([tri_prev, tri_next, tri_prev if q > 0 else z, tri_next if q < 3 else z], axis=1)
    m["masks"] = np.ascontiguousarray(masks).astype(ml_dtypes.bfloat16)
    return m


_NC_CACHE = {}


def kernel(**inputs):
    sh = _prep_shared(inputs)
    in_maps = [_prep_core(inputs, sh, c) for c in range(8)]
    if "nc" not in _NC_CACHE:
        _NC_CACHE["nc"] = build()
    nc = _NC_CACHE["nc"]
    res = run_bass_kernel_spmd(nc, in_maps, core_ids=list(range(8)))
    out = np.zeros((2, SEQ, D), np.float32)
    for c in range(8):
        b, q = c // 4, c % 4
        out[b, q * OWN:(q + 1) * OWN] = np.asarray(res.results[c]["out"], dtype=np.float32)
    return out
```

```python
import contextlib
import numpy as np
import ml_dtypes
import concourse.bass as bass
import concourse.mybir as mybir
from concourse.bass_utils import run_bass_kernel_spmd

F32 = mybir.dt.float32
BF16 = mybir.dt.bfloat16
AF = mybir.ActivationFunctionType
ALU = mybir.AluOpType
AX = mybir.AxisListType

D = 2048
SEQ = 4096
NCTX = 256
NTOK = SEQ + NCTX
OWN = 1024
NEXP = 32
SCALE = 128 ** -0.5
ALPHA = 2.0 ** 0.25
LAM_INIT = 0.2
LN_EPS = 1e-6
SUBLN_EPS = 1e-5

ENGS = ("pe", "act", "dve", "pool", "sp")
SEM_CHUNK = 8000


class Sched:
    def __init__(self, nc, stack, n_dma_sems=8, n_chunks=6):
        self.nc = nc
        self.ops = []
        self.n_dma_sems = n_dma_sems
        self.sems = {}
        for e in ENGS:
            if e == "sp":
                continue
            for k in range(n_chunks):
                self.sems[(e, k)] = stack.enter_context(nc.semaphore("c_%s_%d" % (e, k)))
        for e in ("sp", "pool"):
            for k in range(n_dma_sems):
                self.sems[("d", e, k)] = stack.enter_context(nc.semaphore("d_%s_%d" % (e, k)))
        self.cnt = {e: 0 for e in ENGS}
        self.dma_rr = {e: 0 for e in ENGS}
        self.dma_cnt = {}
        self.waited = {e: {} for e in ENGS}
        self.n_total = 0

    def op(self, eng, fn, reads=(), writes=()):
        self.ops.append(dict(eng=eng, fn=fn, reads=tuple(reads), writes=tuple(writes), dma=False))

    def dma(self, eng, out, in_, reads=(), writes=(), **kw):
        def fn(e, out=out, in_=in_, kw=kw):
            return e.dma_start(out=out, in_=in_, **kw)
        self.ops.append(dict(eng=eng, fn=fn, reads=tuple(reads), writes=tuple(writes), dma=True))

    def flush(self):
        nc = self.nc
        ops = self.ops
        self.ops = []
        self.n_total += len(ops)
        last_w = {}
        readers = {}
        last_reader = {}
        for i, o in enumerate(ops):
            deps = set()
            for r in o["reads"]:
                if r in last_w:
                    deps.add(last_w[r])
                if isinstance(r, str) and r.startswith("ps"):
                    lr = last_reader.get(r)
                    if lr is not None and ops[lr]["eng"] != o["eng"]:
                        deps.add(lr)
                    last_reader[r] = i
            for w in o["writes"]:
                if w in last_w:
                    deps.add(last_w[w])
                for rd in readers.get(w, ()):
                    deps.add(rd)
            deps.discard(i)
            o["deps"] = deps
            for w in o["writes"]:
                last_w[w] = i
                readers[w] = []
                last_reader.pop(w, None)
            for r in o["reads"]:
                if r not in o["writes"]:
                    readers.setdefault(r, []).append(i)
        signal = [False] * len(ops)
        for i, o in enumerate(ops):
            nd = set()
            for d in o["deps"]:
                od = ops[d]
                if (not od["dma"]) and (not o["dma"]) and od["eng"] == "pe" and o["eng"] == "pe":
                    continue
                nd.add(d)
                if not od["dma"]:
                    signal[d] = True
            o["deps"] = nd
        last_comp = {}
        for i, o in enumerate(ops):
            if not o["dma"] and o["fn"] is not None:
                last_comp[o["eng"]] = i
        for e, i in last_comp.items():
            signal[i] = True
        for i, o in enumerate(ops):
            if o["dma"]:
                q = o["eng"]
                s = ("d", q, self.dma_rr[q] % self.n_dma_sems)
                self.dma_rr[q] += 1
                prev = self.dma_cnt.get(s, 0)
                o["sem"] = s
                o["prev"] = prev
                o["val"] = prev + 16
                self.dma_cnt[s] = prev + 16
            elif signal[i]:
                c = self.cnt[o["eng"]]
                self.cnt[o["eng"]] = c + 1
                o["sem"] = (o["eng"], c // SEM_CHUNK)
                o["val"] = c % SEM_CHUNK + 1
        per_eng = {e: [] for e in ENGS}
        waited = self.waited
        for i, o in enumerate(ops):
            F = o["eng"]
            need = {}
            for d in o["deps"]:
                od = ops[d]
                s, v = od["sem"], od["val"]
                if v > need.get(s, 0):
                    need[s] = v
            if o["dma"] and o["prev"] > 0:
                s = o["sem"]
                if o["prev"] > need.get(s, 0):
                    need[s] = o["prev"]
            waits = []
            for s, v in need.items():
                if waited[F].get(s, 0) >= v:
                    continue
                waited[F][s] = v
                waits.append((s, v))
            per_eng[F].append((waits, o, signal[i]))
        final = {}
        for e, i in last_comp.items():
            final[ops[i]["sem"]] = ops[i]["val"]
        for s, v in self.dma_cnt.items():
            final[s] = v
        for F in ENGS:
            waits = []
            for s, v in final.items():
                if waited[F].get(s, 0) >= v:
                    continue
                waited[F][s] = v
                waits.append((s, v))
            per_eng[F].append((waits, dict(fn=None, dma=False), False))
        sems = self.sems
        with nc.Block() as block:
            def run(engname):
                def body(eng):
                    for waits, o, sig in per_eng[engname]:
                        for s, v in waits:
                            eng.wait_ge(sems[s], v)
                        if o["fn"] is None:
                            continue
                        ins = o["fn"](eng)
                        if o["dma"]:
                            ins.then_inc(sems[o["sem"]], 16)
                        elif sig:
                            ins.then_inc(sems[o["sem"]], 1)
                return body

            block.tensor(run("pe"))
            block.scalar(run("act"))
            block.vector(run("dve"))
            block.gpsimd(run("pool"))
            block.sync(run("sp"))


def build(dbg=False, n_exp=NEXP):
    nc = bass.Bass("TRN2", target_bir_lowering=False)

    def din(name, shape, dt=F32):
        return nc.dram_tensor(name, list(shape), dt, kind="ExternalInput").ap()

    def dscr(name, shape, dt):
        if dbg:
            return nc.dram_tensor(name, list(shape), dt, kind="ExternalOutput").ap()
        return nc.dram_tensor(name, list(shape), dt).ap()

    xs = din("xs", [SEQ, D])
    ctxs = din("ctxs", [NCTX, D])
    cs_in = din("cs", [128, 16, 2])
    w_ada_p = din("w_ada_p", [24, 128, 16, 512])
    b_ada_fm = din("b_ada_fm", [128, 96])
    b_ada_row = din("b_ada_row", [1, 6 * D])
    w_in_p = din("w_in_p", [9, 128, 16, 512])
    w_out_p = din("w_out_p", [128, 16, D])
    w_r_in = din("w_r", [128, 16, 36])
    b_r_in = din("b_r", [1, 36])
    wg_p = din("wg_p", [NEXP, 128, 16, 512])
    wu_p = din("wu_p", [NEXP, 128, 16, 512])
    wd_p = din("wd_p", [NEXP, 128, 4, D])
    sink_in = din("sink", [1, 8])
    lamv_in = din("lamv", [1, 512])
    subg_in = din("subln_g", [1, 256])
    ln1g_in = din("ln1_g", [1, D])
    ln1b_in = din("ln1_b", [1, D])
    ln2g_in = din("ln2_g", [1, D])
    ln2b_in = din("ln2_b", [1, D])
    ropeC_in = din("ropeC", [128, SEQ])
    ropeS_in = din("ropeS", [128, SEQ])
    identb_in = din("ident_b", [128, 128], BF16)
    identf_in = din("ident_f", [128, 128])
    pswap_in = din("pswap", [128, 128], BF16)
    masks_in = din("masks", [128, 4, 128], BF16)
    out = nc.dram_tensor("out", [OWN, D], F32, kind="ExternalOutput").ap()

    hT_d = dscr("hT_d", [16, 128, NTOK], BF16)
    QTA_d = dscr("QTA_d", [128, 8, OWN], BF16)
    KTA_d = dscr("KTA_d", [128, 2, 1536], BF16)
    VA_d = dscr("VA_d", [1536, 256], BF16)
    QTB_d = dscr("QTB_d", [128, 8, OWN], BF16)
    KTB_d = dscr("KTB_d", [128, 8, NTOK], BF16)
    VB_d = dscr("VB_d", [NTOK, 1024], BF16)
    AOT_d = dscr("AOT_d", [16, 128, OWN], BF16)
    G_d = dscr("G_d", [2, 128, D], F32)
    X1_d = dscr("X1_d", [OWN, D], F32)
    Wt_d = dscr("Wt_d", [32, OWN], F32)

    with contextlib.ExitStack() as top:
        S = Sched(nc, top)
        banks = [nc.alloc_psum_tensor("bank%d" % k, [128, 512], F32) for k in range(8)]
        PS = ["ps%d" % k for k in range(8)]

        def sbt(stack, name, shape, dt):
            return stack.enter_context(nc.sbuf_tensor("s_" + name, list(shape), dt))

        ident_b = sbt(top, "ident_b", [128, 128], BF16)
        ident_f = sbt(top, "ident_f", [128, 128], F32)
        pswap = sbt(top, "pswap", [128, 128], BF16)
        ones_b = sbt(top, "ones_b", [128, 128], BF16)
        masks = sbt(top, "masks", [128, 4, 128], BF16)
        modFM = sbt(top, "modFM", [128, 96, 2], F32)
        eps_ln = sbt(top, "eps_ln", [128, 1], F32)
        eps_sub = sbt(top, "eps_sub", [128, 1], F32)
        cs_b = sbt(top, "cs_b", [128, 16, 2], BF16)
        cs_s = sbt(top, "cs_s", [128, 16, 2], F32)
        bfm = sbt(top, "bfm", [128, 96], F32)
        ones_f = sbt(top, "ones_f", [128, 128], F32)

        S.dma("sp", ident_b[:], identb_in, writes=["ident_b"])
        S.dma("sp", ident_f[:], identf_in, writes=["ident_f"])
        S.dma("sp", pswap[:], pswap_in, writes=["pswap"])
        S.dma("sp", masks[:], masks_in, writes=["masks"])
        S.op("dve", lambda e: e.memset(ones_b[:], 1.0), writes=["ones_b"])
        S.op("dve", lambda e: e.memset(ones_f[:], 1.0), writes=["ones_f"])
        S.op("dve", lambda e: e.memset(eps_ln[:], LN_EPS), writes=["eps_ln"])
        S.op("dve", lambda e: e.memset(eps_sub[:], SUBLN_EPS), writes=["eps_sub"])

        _uid = [0]

        def uq(tag):
            _uid[0] += 1
            return (tag, _uid[0])

        def mm(outap, lhsT, rhs, start, stop, reads, writes):
            S.op("pe", lambda e: e.matmul(outap, lhsT=lhsT, rhs=rhs, start=start, stop=stop),
                 reads=reads, writes=writes)

        def ada_panel(j, wpan, pfm_ap, psres):
            buf = wpan[j % 2]
            wres = "wpan%d" % (j % 2)
            S.dma("pool", buf[:], w_ada_p[j], writes=[wres])
            for cc in range(4):
                col = j * 4 + cc
                for kc in range(16):
                    mm(pfm_ap[:, col, :], buf[:, kc, cc * 128:(cc + 1) * 128], cs_b[:, kc, :], kc == 0, kc == 15,
                       [wres, "cs_b"], [psres])

        with contextlib.ExitStack() as ph:
            cs_f = sbt(ph, "cs_f", [128, 16, 2], F32)
            wpan = [sbt(ph, "wpan%d" % i, [128, 16, 512], BF16) for i in range(2)]
            S.dma("sp", cs_f[:], cs_in, writes=["cs_f"])
            S.dma("sp", bfm[:], b_ada_fm, writes=["bfm"])
            S.op("act", lambda e: e.activation(out=cs_s[:], in_=cs_f[:], func=AF.Silu), reads=["cs_f"], writes=["cs_s"])
            S.op("dve", lambda e: e.tensor_copy(out=cs_b[:], in_=cs_s[:]), reads=["cs_s"], writes=["cs_b"])
            pfm = banks[0][:, 0:192].rearrange("p (c j) -> p c j", j=2)
            for j in range(8):
                ada_panel(j, wpan, pfm, PS[0])
            S.op("dve", lambda e: e.tensor_tensor(
                out=modFM[:, 0:32, :], in0=pfm[:, 0:32, :],
                in1=bfm[:, 0:32].unsqueeze(2).to_broadcast([128, 32, 2]), op=ALU.add),
                reads=[PS[0], "bfm"], writes=["modFM"])
            S.op("dve", lambda e: e.tensor_scalar_add(out=modFM[:, 16:32, :], in0=modFM[:, 16:32, :], scalar1=1.0),
                 reads=["modFM"], writes=["modFM"])
            S.flush()

        with contextlib.ExitStack() as ph23:
            KT0 = sbt(ph23, "KT0", [128, 2, NTOK], BF16)
            V0 = sbt(ph23, "V0", [128, 34, 257], BF16)
            QT0 = sbt(ph23, "QT0", [128, 2, OWN], BF16)
            with contextlib.ExitStack() as ph12:
                ropeC = sbt(ph12, "ropeC", [128, SEQ], F32)
                ropeS = sbt(ph12, "ropeS", [128, SEQ], F32)
                S.dma("sp", ropeC[:], ropeC_in, writes=["ropeC"])
                S.dma("sp", ropeS[:], ropeS_in, writes=["ropeS"])
                wp = [sbt(ph12, "wp%d" % i, [128, 16, 512], BF16) for i in range(2)]
                hT = [sbt(ph12, "hT%d" % i, [128, 16, 512], BF16) for i in range(2)]
                with contextlib.ExitStack() as ph:
                    xb = [sbt(ph, "xb%d" % i, [128, D], BF16) for i in range(3)]
                    hTt = [sbt(ph, "hTt%d" % i, [128, 16, 512], BF16) for i in range(2)]
                    for t in range(34):
                        src = xs[t * 128:(t + 1) * 128, :] if t < 32 else ctxs[(t - 32) * 128:(t - 31) * 128, :]
                        jj = 0 if t < 32 else 1
                        xr = "xb%d" % (t % 3)
                        xbt = xb[t % 3]
                        S.dma("pool", xbt[:], src, writes=[xr])
                        g, sub = t // 4, t % 4
                        hb = hTt[g % 2]
                        kA, kB = (t % 2) * 2, (t % 2) * 2 + 1
                        pa = banks[kA][:].bitcast(BF16).rearrange("p (k t) -> p k t", t=128)
                        pb = banks[kB][:].bitcast(BF16).rearrange("p (k t) -> p k t", t=128)
                        for kc in range(16):
                            pp, kk = (pa, kA) if kc < 8 else (pb, kB)
                            S.op("pe", lambda e, pp=pp, kc=kc, xbt=xbt: e.transpose(pp[:, kc % 8, :], xbt[:, kc * 128:(kc + 1) * 128], ident_b[:]),
                                 reads=[xr, "ident_b"], writes=[PS[kk]])
                        hres_a = ("hTt", g % 2, sub, 0)
                        hres_b = ("hTt", g % 2, sub, 1)
                        for kc in range(8):
                            S.op("act", lambda e, kc=kc, hb=hb, pa=pa, sub=sub, jj=jj: e.activation(
                                out=hb[:, kc, sub * 128:(sub + 1) * 128], in_=pa[:, kc, :], func=AF.Identity,
                                bias=modFM[:, kc, jj:jj + 1], scale=modFM[:, 16 + kc, jj:jj + 1]),
                                reads=[PS[kA], "modFM"], writes=[hres_a])
                        for kc in range(8, 16):
                            S.op("dve", lambda e, kc=kc, hb=hb, pb=pb, sub=sub, jj=jj: e.tensor_scalar(
                                out=hb[:, kc, sub * 128:(sub + 1) * 128], in0=pb[:, kc - 8, :],
                                scalar1=modFM[:, 16 + kc, jj:jj + 1], scalar2=modFM[:, kc, jj:jj + 1],
                                op0=ALU.mult, op1=ALU.add),
                                reads=[PS[kB], "modFM"], writes=[hres_b])
                        if sub == 3 or t == 33:
                            ntok = (sub + 1) * 128
                            base = g * 512
                            S.dma("sp", hT_d[:, :, base:base + ntok].rearrange("k p t -> p k t"), hb[:, :, 0:ntok],
                                  reads=[("hTt", g % 2, s_, ab) for s_ in range(sub + 1) for ab in (0, 1)],
                                  writes=["hT_d_g0" if g == 0 else uq("hT_d")])
                    S.dma("pool", wp[0][:], w_in_p[0], writes=["wp0_pre"])
                    S.dma("sp", hT[0][:, :, 0:512], hT_d[:, :, 0:512].rearrange("k p t -> p k t"), reads=["hT_d_g0"], writes=["hT0_pre"])
                    S.flush()

                with contextlib.ExitStack() as ph:
                    wp = wp + [sbt(ph, "wp%d" % i, [128, 16, 512], BF16) for i in (2, 3)]
                    xb16 = [sbt(ph, "xb16_%d" % i, [128, 512], BF16) for i in range(2)]
                    t1 = [sbt(ph, "t1_%d" % i, [128, 512], F32) for i in range(2)]
                    t2 = [sbt(ph, "t2_%d" % i, [128, 512], F32) for i in range(2)]
                    so = [sbt(ph, "so%d" % i, [128, 4, 512], BF16) for i in range(2)]
                    sv = [sbt(ph, "sv%d" % i, [128, 512], BF16) for i in range(2)]
                    LT = [(i * 512, 512, True) for i in range(8)]
                    HALO = (1024, 256, True)
                    CTX = (SEQ, 256, False)
                    plan = [
                        (0, "FM", QTA_d, 0, LT[0:2]), (1, "FM", QTA_d, 4, LT[0:2]),
                        (2, "KAVA", None, 0, [LT[0], LT[1], HALO, CTX]),
                        (3, "FM", QTB_d, 0, LT[0:2]), (4, "FM", QTB_d, 4, LT[0:2]),
                        (5, "FM", KTB_d, 0, LT + [CTX]), (6, "FM", KTB_d, 4, LT + [CTX]),
                        (7, "TM", VB_d, 0, LT + [CTX]), (8, "TM", VB_d, 512, LT + [CTX]),
                    ]
                    items = []
                    hcnt = 0
                    for pi, (pn, kind, dest, slot0, tiles) in enumerate(plan[0:5]):
                        for ti, tl in enumerate(tiles):
                            items.append(dict(pn=pn, kind=kind, dest=dest, slot0=slot0, tile=tl, wb=pi % 2, load_w=(ti == 0), hb=hcnt % 2, load_h=True))
                            hcnt += 1
                    wbmap = {5: 2, 6: 3, 7: 1, 8: 0}
                    for ti, tl in enumerate(LT + [CTX]):
                        for k_, pidx in enumerate((5, 6, 7)):
                            pn, kind, dest, slot0, _ = plan[pidx]
                            items.append(dict(pn=pn, kind=kind, dest=dest, slot0=slot0, tile=tl, wb=wbmap[pidx], load_w=(ti == 0), hb=hcnt % 2, load_h=(k_ == 0)))
                        hcnt += 1
                    for ti, tl in enumerate(LT + [CTX]):
                        pn, kind, dest, slot0, _ = plan[8]
                        items.append(dict(pn=pn, kind=kind, dest=dest, slot0=slot0, tile=tl, wb=wbmap[8], load_w=(ti == 0), hb=hcnt % 2, load_h=True))
                        hcnt += 1
                    cst = dict(so=0, fm=0, tm=0)
                    stores = {pn_: [] for pn_ in range(9)}
                    h_loads = [i for i, it_ in enumerate(items) if it_["load_h"]]
                    hptr = [1]
                    tile_ord = []
                    for it_ in items:
                        tile_ord.append((tile_ord[-1] if tile_ord else -1) + (1 if it_["load_h"] else 0))

                    def load_item(i):
                        it_ = items[i]
                        if it_["load_w"] and i > 0:
                            S.dma("pool", wp[it_["wb"]][:], w_in_p[it_["pn"]], writes=["wp%d" % it_["wb"]])
                        while hptr[0] < len(h_loads) and i >= 1 and tile_ord[i - 1] >= hptr[0] - 1:
                            j_ = h_loads[hptr[0]]
                            hptr[0] += 1
                            start_, ntok_, _l = items[j_]["tile"]
                            hb_ = items[j_]["hb"]
                            S.dma("sp", hT[hb_][:, :, 0:ntok_], hT_d[:, :, start_:start_ + ntok_].rearrange("k p t -> p k t"),
                                  writes=["hT%d" % hb_])

                    def compute_item(i):
                        it_ = items[i]
                        pn, kind, dest, slot0 = it_["pn"], it_["kind"], it_["dest"], it_["slot0"]
                        start, ntok, latent = it_["tile"]
                        wbuf, wres = wp[it_["wb"]], "wp%d" % it_["wb"]
                        hbuf, hres = hT[it_["hb"]], "hT%d" % it_["hb"]
                        ka_tok = start if start < OWN else (1024 if start == 1024 else 1280)
                        fm_chunks = {"FM": 4, "KAVA": 2, "TM": 0}[kind]
                        if fm_chunks:
                            sob = so[cst["so"] % 2]
                            sores = "so%d" % (cst["so"] % 2)
                            cst["so"] += 1
                            kqs = []

                            def mm_chunk(cc):
                                kq = cst["fm"] % 2
                                cst["fm"] += 1
                                kqs.append(kq)
                                for kc in range(16):
                                    mm(banks[kq][:, 0:ntok], wbuf[:, kc, cc * 128:(cc + 1) * 128], hbuf[:, kc, 0:ntok], kc == 0, kc == 15,
                                       [wres, hres], [PS[kq]])

                            def post_chunk(cc):
                                kq = kqs[cc]
                                pq, psw = banks[kq], banks[2 + kq]
                                if latent:
                                    xbb, t1b, t2b = xb16[kq], t1[kq], t2[kq]
                                    S.op("act", lambda e, xbb=xbb, pq=pq, ntok=ntok: e.activation(out=xbb[:, 0:ntok], in_=pq[:, 0:ntok], func=AF.Copy),
                                         reads=[PS[kq]], writes=["xb16_%d" % kq])
                                    mm(psw[:, 0:ntok], pswap[:], xbb[:, 0:ntok], True, True, ["pswap", "xb16_%d" % kq], [PS[2 + kq]])
                                    S.op("dve", lambda e, t1b=t1b, pq=pq, ntok=ntok, start=start: e.tensor_tensor(
                                        out=t1b[:, 0:ntok], in0=pq[:, 0:ntok], in1=ropeC[:, start:start + ntok], op=ALU.mult),
                                        reads=[PS[kq], "ropeC"], writes=["t1_%d" % kq])
                                    S.op("dve", lambda e, t2b=t2b, psw=psw, ntok=ntok, start=start: e.tensor_tensor(
                                        out=t2b[:, 0:ntok], in0=psw[:, 0:ntok], in1=ropeS[:, start:start + ntok], op=ALU.mult),
                                        reads=[PS[2 + kq], "ropeS"], writes=["t2_%d" % kq])
                                    S.op("dve", lambda e, sob=sob, cc=cc, t1b=t1b, t2b=t2b, ntok=ntok: e.tensor_tensor(
                                        out=sob[:, cc, 0:ntok], in0=t1b[:, 0:ntok], in1=t2b[:, 0:ntok], op=ALU.add),
                                        reads=["t1_%d" % kq, "t2_%d" % kq], writes=[(sores, cc)])
                                else:
                                    S.op("act", lambda e, sob=sob, cc=cc, pq=pq, ntok=ntok: e.activation(out=sob[:, cc, 0:ntok], in_=pq[:, 0:ntok], func=AF.Copy),
                                         reads=[PS[kq]], writes=[(sores, cc)])

                            mm_chunk(0)
                            for cc in range(fm_chunks):
                                if cc + 1 < fm_chunks:
                                    mm_chunk(cc + 1)
                                post_chunk(cc)
                            if kind == "FM":
                                dst = dest[:, slot0:slot0 + 4, start:start + ntok]
                            else:
                                dst = KTA_d[:, 0:2, ka_tok:ka_tok + ntok]
                            nm = uq("fm_out")
                            stores[pn].append(nm)
                            S.dma("sp", dst, sob[:, 0:fm_chunks, 0:ntok], reads=[(sores, c_) for c_ in range(fm_chunks)],
                                  writes=[nm])
                        if kind in ("TM", "KAVA"):
                            c0, ncols = (0, 512) if kind == "TM" else (256, 256)
                            for ts in range(ntok // 128):
                                kv_ = cst["tm"] % 2
                                cst["tm"] += 1
                                pv = banks[4 + kv_]
                                svb = sv[kv_]
                                for kc in range(16):
                                    mm(pv[:, 0:ncols], hbuf[:, kc, ts * 128:(ts + 1) * 128], wbuf[:, kc, c0:c0 + ncols], kc == 0, kc == 15,
                                       [wres, hres], [PS[4 + kv_]])
                                if kv_ == 0:
                                    S.op("act", lambda e, svb=svb, pv=pv, ncols=ncols: e.activation(out=svb[:, 0:ncols], in_=pv[:, 0:ncols], func=AF.Copy),
                                         reads=[PS[4]], writes=["sv0"])
                                else:
                                    S.op("dve", lambda e, svb=svb, pv=pv, ncols=ncols: e.tensor_copy(out=svb[:, 0:ncols], in_=pv[:, 0:ncols]),
                                         reads=[PS[5]], writes=["sv1"])
                                if kind == "TM":
                                    r0 = start + ts * 128
                                    dst = VB_d[r0:r0 + 128, slot0:slot0 + 512]
                                else:
                                    r0 = ka_tok + ts * 128
                                    dst = VA_d[r0:r0 + 128, 0:256]
                                nm = uq("tm_out")
                                stores[pn].append(nm)
                                S.dma("sp", dst, svb[:, 0:ncols], reads=["sv%d" % kv_], writes=[nm])

                    load_item(0)
                    for i in range(len(items)):
                        if i + 1 < len(items):
                            load_item(i + 1)
                        if items[i]["pn"] == 8 and items[i]["load_w"]:
                            S.dma("sp", KT0[:], KTB_d[:, 0:2, :], reads=stores[5], writes=["KT0_pre"])
                            for half in range(2):
                                S.dma("sp", V0[:, half * 17:(half + 1) * 17, 0:256],
                                      VB_d[half * 2176:(half + 1) * 2176, 0:256].rearrange("(kt p) c -> p kt c", p=128),
                                      reads=stores[7], writes=["V0_pre%d" % half])
                            S.dma("sp", QT0[:], QTB_d[:, 0:2, :], reads=stores[3], writes=["QT0_pre"])
                        compute_item(i)
                    S.flush()

            with contextlib.ExitStack() as ph:
                KT = [KT0, sbt(ph, "KT1", [128, 2, NTOK], BF16)]
                V = [V0, sbt(ph, "V1", [128, 34, 257], BF16)]
                QT = [QT0, sbt(ph, "QT1", [128, 2, OWN], BF16)]
                NE = 4
                E = [sbt(ph, "E%d" % i, [128, 512], BF16) for i in range(NE)]
                n1 = sbt(ph, "n1", [128, 4, 256], F32)
                dd = [sbt(ph, "dd%d" % i, [128, 256], F32) for i in range(4)]
                sq4 = [sbt(ph, "sq%d" % i, [128, 256], F32) for i in range(4)]
                ss = [sbt(ph, "ss%d" % i, [128, 4], F32) for i in range(4)]
                rz = [sbt(ph, "rz%d" % i, [128, 1], F32) for i in range(4)]
                gsub = sbt(ph, "gsub", [128, 256], F32)
                lamb = sbt(ph, "lamb", [128, 512], F32)
                lt = sbt(ph, "lt", [128, 8], F32)
                ob = [sbt(ph, "ob%d" % i, [128, 256], BF16) for i in range(4)]
                oT = [sbt(ph, "oT%d" % i, [128, 2, 128], BF16) for i in range(4)]
                wpan = [sbt(ph, "wpanB%d" % i, [128, 16, 512], BF16) for i in range(2)]
                pfm3 = banks[6][:, 256:448].rearrange("p (c j) -> p c j", j=2)
                neglam = lt[:, 6:7]
                for i in range(2):
                    S.op("dve", lambda e, i=i: e.memset(V[i][:, :, 256:257], 1.0), writes=[("V1", i)])
                S.dma("sp", gsub[:], subg_in.partition_broadcast(128), writes=["gsub"])
                S.op("dve", lambda e: e.tensor_scalar_mul(out=gsub[:], in0=gsub[:], scalar1=1.0 - LAM_INIT), reads=["gsub"], writes=["gsub"])
                S.dma("sp", lamb[:], lamv_in.partition_broadcast(128), writes=["lamb"])
                for i in range(2):
                    S.op("dve", lambda e, i=i: e.tensor_tensor(out=lamb[:, i * 256:i * 256 + 128], in0=lamb[:, i * 256:i * 256 + 128],
                                                             in1=lamb[:, i * 256 + 128:i * 256 + 256], op=ALU.mult),
                         reads=["lamb"], writes=["lamb"])
                    S.op("dve", lambda e, i=i: e.reduce_sum(out=lt[:, i:i + 1], in_=lamb[:, i * 256:i * 256 + 128], axis=AX.X),
                         reads=["lamb"], writes=["lt"])
                S.op("act", lambda e: e.activation(out=lt[:, 2:4], in_=lt[:, 0:2], func=AF.Exp), reads=["lt"], writes=["lt"])
                S.op("dve", lambda e: e.tensor_tensor(out=lt[:, 5:6], in0=lt[:, 3:4], in1=lt[:, 2:3], op=ALU.subtract), reads=["lt"], writes=["lt"])
                S.op("dve", lambda e: e.tensor_scalar_add(out=lt[:, 6:7], in0=lt[:, 5:6], scalar1=-LAM_INIT), reads=["lt"], writes=["lt"])

                def load_head(h):
                    hb_ = h % 2
                    S.dma("sp", KT[hb_][:], KTB_d[:, 2 * h:2 * h + 2, :], writes=[("KT", hb_)])
                    for half in range(2):
                        S.dma("sp", V[hb_][:, half * 17:(half + 1) * 17, 0:256],
                              VB_d[half * 2176:(half + 1) * 2176, h * 256:(h + 1) * 256].rearrange("(kt p) c -> p kt c", p=128),
                              writes=[("V", hb_, half)])
                    S.dma("sp", QT[hb_][:], QTB_d[:, 2 * h:2 * h + 2, :], writes=[("QT", hb_)])

                steps = [(h, qt, r, kt) for h in range(4) for qt in range(2) for r in range(2) for kt in range(34)]
                SB = (4, 5, 7)

                def emit_S(i):
                    h, qt, r, kt = steps[i]
                    hb_ = h % 2
                    ks = SB[i % 3]
                    mm(banks[ks][:], KT[hb_][:, r, kt * 128:(kt + 1) * 128], QT[hb_][:, r, qt * 512:(qt + 1) * 512], True, True,
                       [("KT", hb_), ("QT", hb_)], [PS[ks]])

                cntO = 0
                pending = []
                staged = []

                ada_q = []
                accs = [sbt(ph, "acc%d" % i, [128, 512], F32) for i in range(2)]

                def ada_gemv(jp):
                    buf, wres = wpan[jp % 2], "wpanB%d" % (jp % 2)
                    acc, ares = accs[jp % 2], "acc%d" % (jp % 2)
                    S.op("dve", lambda e: e.tensor_scalar(out=acc[:], in0=buf[:, 0, :], scalar1=cs_s[:, 0, 0:1], scalar2=None, op0=ALU.mult),
                         reads=[wres, "cs_s"], writes=[ares])
                    for kc in range(1, 16):
                        S.op("dve", lambda e, kc=kc: e.scalar_tensor_tensor(out=acc[:], in0=buf[:, kc, :], scalar=cs_s[:, kc, 0:1], in1=acc[:],
                                                                          op0=ALU.mult, op1=ALU.add),
                             reads=[wres, "cs_s", ares], writes=[ares])

                def ada_reduce(jp):
                    acc, ares = accs[jp % 2], "acc%d" % (jp % 2)
                    for cc in range(4):
                        mm(pfm3[:, jp * 4 + cc, :], acc[:, cc * 128:(cc + 1) * 128], ones_f[:, 0:2], True, True, [ares, "ones_f"], [PS[6]])

                def run_stage(k):
                    if not staged:
                        return
                    g = staged[0]
                    for qs, o_ in g["chain"]:
                        ddb, obb, ssb = dd[o_], ob[o_], ss[o_]
                        if k == 0:
                            S.op("dve", lambda e, qs=qs, ddb=ddb: e.tensor_tensor(out=ddb[:], in0=ddb[:], in1=n1[:, qs, :], op=ALU.add),
                                 reads=["dd%d" % o_, ("n1", qs)], writes=["dd%d" % o_])
                            S.op("dve", lambda e, ddb=ddb, sqb=sq4[qs]: e.tensor_tensor(out=sqb[:], in0=ddb[:], in1=ddb[:], op=ALU.mult),
                                 reads=["dd%d" % o_], writes=["sq%d" % qs])
                            S.op("dve", lambda e, ssb=ssb, sqb=sq4[qs]: e.reduce_sum(out=ssb[:, 0:1], in_=sqb[:], axis=AX.X),
                                 reads=["sq%d" % qs], writes=["ss%d" % o_])
                        elif k == 1:
                            S.op("act", lambda e, ssb=ssb: e.activation(out=ssb[:, 1:2], in_=ssb[:, 0:1], func=AF.Ln,
                                                                      bias=eps_sub[:, 0:1], scale=1.0 / 256.0),
                                 reads=["ss%d" % o_, "eps_sub"], writes=["ss%d" % o_])
                            S.op("act", lambda e, ssb=ssb: e.activation(out=ssb[:, 2:3], in_=ssb[:, 1:2], func=AF.Exp, scale=-0.5),
                                 reads=["ss%d" % o_], writes=["ss%d" % o_])
                        elif k == 2:
                            S.op("dve", lambda e, ssb=ssb, ddb=ddb, obb=obb: e.scalar_tensor_tensor(
                                out=obb[:], in0=ddb[:], scalar=ssb[:, 2:3], in1=gsub[:], op0=ALU.mult, op1=ALU.mult),
                                reads=["dd%d" % o_, "ss%d" % o_, "gsub"], writes=["ob%d" % o_])
                            pending.append((o_, g["h"], g["qt"] * 512 + qs * 128))
                    if k == 2:
                        staged.pop(0)

                def flush_pending():
                    while pending:
                        o_, h_, tok0 = pending.pop(0)
                        obb, oTb = ob[o_], oT[o_]
                        pT = banks[6][:].bitcast(BF16)[:, (o_ % 2) * 256:(o_ % 2 + 1) * 256].rearrange("p (c t) -> p c t", t=128)
                        for c_ in range(2):
                            S.op("pe", lambda e, pT=pT, c_=c_, obb=obb: e.transpose(pT[:, c_, :], obb[:, c_ * 128:(c_ + 1) * 128], ident_b[:]),
                                 reads=["ob%d" % o_, "ident_b"], writes=[PS[6]])
                        S.op("act", lambda e, pT=pT, oTb=oTb: e.activation(out=oTb[:], in_=pT, func=AF.Copy),
                             reads=[PS[6]], writes=["oT%d" % o_])
                        S.dma("sp", AOT_d[8 + 2 * h_:10 + 2 * h_, :, tok0:tok0 + 128].rearrange("c p t -> p c t"), oTb[:],
                              reads=["oT%d" % o_], writes=[uq("AOT_d")])

                emit_S(0)
                emit_S(1)
                for i, (h, qt, r, kt) in enumerate(steps):
                    hb_ = h % 2
                    Vb = V[hb_]
                    if kt == 20 and ada_q:
                        ada_gemv(ada_q[0])
                    if kt == 30 and ada_q:
                        ada_reduce(ada_q.pop(0))
                    if kt == 3:
                        run_stage(0)
                    elif kt == 7:
                        run_stage(1)
                    elif kt == 11:
                        run_stage(2)
                    elif kt == 15:
                        flush_pending()
                    if qt == 0 and r == 0 and kt == 0 and h + 1 < 4:
                        load_head(h + 1)
                    if i + 2 < len(steps):
                        emit_S(i + 2)
                    ks = SB[i % 3]
                    Eb = E[i % NE]
                    eres = "E%d" % (i % NE)
                    S.op("act", lambda e, Eb=Eb, ks=ks: e.activation(out=Eb[:], in_=banks[ks][:], func=AF.Exp, scale=SCALE),
                         reads=[PS[ks]], writes=[eres])
                    for qs in range(4):
                        mm(banks[qs][:, 0:257], Eb[:, qs * 128:(qs + 1) * 128], Vb[:, kt, :], kt == 0, kt == 33,
                           [eres, ("V", hb_, kt // 17), ("V1", hb_)], [PS[qs]])
                    if kt != 33:
                        continue
                    chain = []
                    for qs in range(4):
                        S.op("dve", lambda e, qs=qs: e.reciprocal(out=rz[qs][:], in_=banks[qs][:, 256:257]),
                             reads=[PS[qs]], writes=["rz%d" % qs])
                        if r == 0:
                            S.op("dve", lambda e, qs=qs: e.tensor_scalar(out=n1[:, qs, :], in0=banks[qs][:, 0:256], scalar1=rz[qs][:, 0:1],
                                                                      scalar2=None, op0=ALU.mult),
                                 reads=[PS[qs], "rz%d" % qs], writes=[("n1", qs)])
                        else:
                            o_ = cntO % 4
                            cntO += 1
                            ddb = dd[o_]
                            S.op("dve", lambda e, qs=qs, ddb=ddb: e.tensor_scalar(out=ddb[:], in0=banks[qs][:, 0:256], scalar1=rz[qs][:, 0:1],
                                                                                 scalar2=neglam, op0=ALU.mult, op1=ALU.mult),
                                 reads=[PS[qs], "rz%d" % qs, "lt"], writes=["dd%d" % o_])
                            chain.append((qs, o_))
                    if chain:
                        staged.append(dict(chain=list(chain), h=h, qt=qt))
                    jp = 8 + (h * 4 + qt * 2 + r)
                    S.dma("pool", wpan[jp % 2][:], w_ada_p[jp], writes=["wpanB%d" % (jp % 2)])
                    ada_q.append(jp)
                while ada_q:
                    ada_gemv(ada_q[0])
                    ada_reduce(ada_q.pop(0))
                for k_ in range(3):
                    run_stage(k_)
                flush_pending()
                S.flush()

        with contextlib.ExitStack() as ph56:
            h2Tb = sbt(ph56, "h2Tb", [128, 16, OWN], BF16)

            def layer_norm(src, dst, gtile, btile, stt, tagres):
                (sap, sres), (dap, dres) = src, dst
                sres = list(sres) if isinstance(sres, list) else [sres]
                st6, mv = stt
                for c in range(4):
                    S.op("dve", lambda e, c=c: e.bn_stats(out=st6[:, c, :], in_=sap[:, c * 512:(c + 1) * 512]),
                         reads=sres, writes=[tagres + "st"])
                S.op("dve", lambda e: e.bn_aggr(out=mv[:, 0:2], in_=st6[:]), reads=[tagres + "st"], writes=[tagres + "mv"])
                S.op("act", lambda e: e.activation(out=mv[:, 2:3], in_=mv[:, 1:2], func=AF.Ln, bias=eps_ln[:, 0:1], scale=1.0),
                     reads=[tagres + "mv", "eps_ln"], writes=[tagres + "mv"])
                S.op("act", lambda e: e.activation(out=mv[:, 3:4], in_=mv[:, 2:3], func=AF.Exp, scale=-0.5),
                     reads=[tagres + "mv"], writes=[tagres + "mv"])
                S.op("dve", lambda e: e.scalar_tensor_tensor(out=dap, in0=sap, scalar=mv[:, 0:1], in1=gtile[0], op0=ALU.subtract, op1=ALU.mult),
                     reads=sres + [tagres + "mv", gtile[1]], writes=[dres])
                S.op("dve", lambda e: e.scalar_tensor_tensor(out=dap, in0=dap, scalar=mv[:, 3:4], in1=btile[0], op0=ALU.mult, op1=ALU.add),
                     reads=[dres, tagres + "mv", btile[1]], writes=[dres])

            with contextlib.ExitStack() as phwo:
                wo = sbt(phwo, "wo", [128, 16, D], BF16)
                with contextlib.ExitStack() as ph:
                    QTA = sbt(ph, "QTA", [128, 8, OWN], BF16)
                    KTA = sbt(ph, "KTA", [128, 2, 1536], BF16)
                    VA = sbt(ph, "VA", [128, 12, 256], BF16)
                    sinkb = sbt(ph, "sinkb", [128, 8], F32)
                    sexp = sbt(ph, "sexp", [128, 8], F32)
                    sinkexp = sbt(ph, "sinkexp", [128, 8, 128], F32)
                    EA = [sbt(ph, "EA%d" % i, [128, 4, 128], BF16) for i in range(3)]
                    den = sbt(ph, "den", [128, 512], F32)
                    oa = [sbt(ph, "oa%d" % i, [128, 4, 128], BF16) for i in range(2)]
                    S.dma("sp", QTA[:], QTA_d, writes=["QTA"])
                    S.dma("sp", KTA[:], KTA_d, writes=["KTA"])
                    S.dma("sp", VA[:], VA_d.rearrange("(kt p) c -> p kt c", p=128), writes=["VA"])
                    for q4 in range(4):
                        S.dma("pool", wo[:, q4 * 4:(q4 + 1) * 4, :], w_out_p[:, q4 * 4:(q4 + 1) * 4, :], reads=["QTA", "KTA", "VA"], writes=[("wo", q4)])
                    S.dma("sp", sinkb[:], sink_in.partition_broadcast(128), writes=["sinkb"])
                    S.op("act", lambda e: e.activation(out=sexp[:], in_=sinkb[:], func=AF.Exp), reads=["sinkb"], writes=["sexp"])
                    S.op("dve", lambda e: e.tensor_copy(out=sinkexp[:], in_=sexp[:].unsqueeze(2).to_broadcast([128, 8, 128])),
                         reads=["sexp"], writes=["sinkexp"])
                    dg = [sbt(ph, "dg%d" % i, [128, 4, 128], F32) for i in range(2)]
                    gb = sbt(ph, "gb", [128, D], F32)
                    pfm3 = banks[6][:, 256:448].rearrange("p (c j) -> p c j", j=2)
                    S.op("dve", lambda e: e.tensor_tensor(
                        out=modFM[:, 32:96, :], in0=pfm3[:, 32:96, :],
                        in1=bfm[:, 32:96].unsqueeze(2).to_broadcast([128, 64, 2]), op=ALU.add),
                        reads=[PS[6], "bfm"], writes=["modFM"])
                    S.op("dve", lambda e: e.tensor_scalar_add(out=modFM[:, 64:80, :], in0=modFM[:, 64:80, :], scalar1=1.0),
                         reads=["modFM"], writes=["modFM"])
                    for gi, base in ((0, 32), (1, 80)):
                        for q4 in range(4):
                            dgb = dg[q4 % 2]
                            dres = "dg%d" % (q4 % 2)
                            S.op("dve", lambda e, dgb=dgb, base=base, q4=q4: e.tensor_tensor(
                                out=dgb[:], in0=ident_f[:].unsqueeze(1).to_broadcast([128, 4, 128]),
                                in1=modFM[:, base + 4 * q4:base + 4 * q4 + 4, 0:1].to_broadcast([128, 4, 128]), op=ALU.mult),
                                reads=["ident_f", "modFM"], writes=[dres])
                            mm(banks[q4][:], ones_f[:], dgb[:].rearrange("p a b -> p (a b)"), True, True, ["ones_f", dres], [PS[q4]])
                        for c in range(4):
                            S.op("act", lambda e, c=c: e.activation(out=gb[:, c * 512:(c + 1) * 512], in_=banks[c][:], func=AF.Copy),
                                 reads=[PS[c]], writes=[("gb", c)])
                        S.dma("sp", G_d[gi], gb[:], reads=[("gb", c) for c in range(4)], writes=[uq("G_d")])
                    NEA = 4
                    EA4 = EA + [sbt(ph, "EA3", [128, 4, 128], BF16)]
                    steps = []
                    for n in range(8):
                        for kv in range(2):
                            keys = [((n - 1) if n > 0 else 8, 0 if n > 0 else 2), (n, None),
                                    ((n + 1) if n < 7 else 9, 1 if n < 7 else 3), (10, None), (11, None)]
                            for j, (ktile, m) in enumerate(keys):
                                steps.append((n, kv, j, ktile, m))
                    SB = (4, 5, 6)

                    def emit_SA(i):
                        n, kv, j, ktile, m = steps[i]
                        ks = SB[i % 3]
                        mm(banks[ks][:], KTA[:, kv, ktile * 128:(ktile + 1) * 128], QTA[:, kv * 4:(kv + 1) * 4, n * 128:(n + 1) * 128], True, True,
                           ["KTA", "QTA"], [PS[ks]])

                    emit_SA(0)
                    emit_SA(1)
                    for i, (n, kv, j, ktile, m) in enumerate(steps):
                        it = n * 2 + kv
                        kO, kZ = (it % 2) * 2, (it % 2) * 2 + 1
                        oab = oa[it % 2]
                        oares = "oa%d" % (it % 2)
                        if i + 2 < len(steps):
                            emit_SA(i + 2)
                        ks = SB[i % 3]
                        Eb = EA4[i % NEA]
                        eres = "EA%d" % (i % NEA)
                        pS3 = banks[ks][:].rearrange("p (g t) -> p g t", t=128)
                        S.op("act", lambda e, Eb=Eb, pS3=pS3: e.activation(out=Eb[:], in_=pS3, func=AF.Exp, scale=SCALE),
                             reads=[PS[ks]], writes=[eres])
                        if m is not None:
                            S.op("dve", lambda e, Eb=Eb, m=m: e.tensor_tensor(out=Eb[:], in0=Eb[:], in1=masks[:, m:m + 1, :].to_broadcast([128, 4, 128]),
                                                                            op=ALU.mult),
                                 reads=[eres, "masks"], writes=[eres])
                        mm(banks[kO][:], VA[:, ktile, kv * 128:(kv + 1) * 128], Eb[:], j == 0, j == 4, ["VA", eres], [PS[kO]])
                        mm(banks[kZ][:], ones_b[:], Eb[:], j == 0, j == 4, ["ones_b", eres], [PS[kZ]])
                        if j != 4:
                            continue
                        S.op("dve", lambda e, kZ=kZ, kv=kv: e.tensor_tensor(out=den[:], in0=banks[kZ][:],
                                                                          in1=sinkexp[:, kv * 4:(kv + 1) * 4, :].rearrange("p g t -> p (g t)"), op=ALU.add),
                             reads=[PS[kZ], "sinkexp"], writes=["den"])
                        S.op("act", lambda e: e.activation(out=den[:], in_=den[:], func=AF.Ln), reads=["den"], writes=["den"])
                        S.op("act", lambda e: e.activation(out=den[:], in_=den[:], func=AF.Exp, scale=-1.0), reads=["den"], writes=["den"])
                        S.op("dve", lambda e, kO=kO, oab=oab: e.tensor_tensor(out=oab[:].rearrange("p g t -> p (g t)"), in0=banks[kO][:], in1=den[:], op=ALU.mult),
                             reads=[PS[kO], "den"], writes=[oares])
                        S.dma("sp", AOT_d[kv * 4:(kv + 1) * 4, :, n * 128:(n + 1) * 128].rearrange("c p t -> p c t"), oab[:],
                              reads=[oares], writes=[uq("AOT_dA")])
                    S.flush()

                with contextlib.ExitStack() as ph:
                    G1B = sbt(ph, "G1B", [128, D], F32)
                    l1g = sbt(ph, "l1g", [128, D], F32)
                    l1b = sbt(ph, "l1b", [128, D], F32)
                    wr = sbt(ph, "wr", [128, 16, 36], F32)
                    brb = sbt(ph, "brb", [128, 36], F32)
                    aot = [sbt(ph, "aot%d" % i, [128, 16, 128], BF16) for i in range(2)]
                    xt = [sbt(ph, "xt%d" % i, [128, D], F32) for i in range(2)]
                    rr = sbt(ph, "rr", [128, D], F32)
                    x1 = [sbt(ph, "x1_%d" % i, [128, D], F32) for i in range(2)]
                    h2Tf = sbt(ph, "h2Tf", [128, 16, 128], F32)
                    st6 = sbt(ph, "st6", [128, 4, 6], F32)
                    mv = sbt(ph, "mv", [128, 4], F32)
                    rt = sbt(ph, "rt", [128, 64], F32)
                    lg = sbt(ph, "lg", [128, 36], F32)
                    lm = sbt(ph, "lm", [128, 32], F32)
                    s1 = sbt(ph, "s1", [128, 32], F32)
                    s2 = sbt(ph, "s2", [128, 32], F32)
                    m8 = sbt(ph, "m8", [128, 8], F32)
                    wtT = sbt(ph, "wtT", [32, 128], F32)
                    S.dma("sp", G1B[:], G_d[0], writes=["G1B"])
                    S.dma("sp", l1g[:], ln1g_in.partition_broadcast(128), writes=["l1g"])
                    S.dma("sp", l1b[:], ln1b_in.partition_broadcast(128), writes=["l1b"])
                    S.dma("sp", wr[:], w_r_in, writes=["wr"])
                    S.dma("sp", brb[:], b_r_in.partition_broadcast(128), writes=["brb"])
                    def load5(t):
                        S.dma("sp", aot[t % 2][:], AOT_d[:, :, t * 128:(t + 1) * 128].rearrange("k p t -> p k t"), writes=["aot%d" % (t % 2)])
                        S.dma("sp", xt[t % 2][:], xs[t * 128:(t + 1) * 128, :], writes=["xt%d" % (t % 2)])

                    def stageA(t):
                        ab, xtb, x1b = aot[t % 2], xt[t % 2], x1[t % 2]
                        ares, xres, x1res = "aot%d" % (t % 2), "xt%d" % (t % 2), "x1_%d" % (t % 2)
                        for c in range(4):
                            for kc in range(16):
                                mm(banks[c][:], ab[:, kc, :], wo[:, kc, c * 512:(c + 1) * 512], kc == 0, kc == 15,
                                   [ares, ("wo", kc // 4)], [PS[c]])

                    def stageA2(t):
                        ab, xtb, x1b = aot[t % 2], xt[t % 2], x1[t % 2]
                        ares, xres, x1res = "aot%d" % (t % 2), "xt%d" % (t % 2), "x1_%d" % (t % 2)
                        for c in range(4):
                            S.op("dve", lambda e, c=c: e.tensor_tensor(out=rr[:, c * 512:(c + 1) * 512], in0=banks[c][:],
                                                                     in1=G1B[:, c * 512:(c + 1) * 512], op=ALU.mult),
                                 reads=[PS[c], "G1B"], writes=[("rr", c)])
                        S.op("dve", lambda e, xtb=xtb: e.scalar_tensor_tensor(out=rr[:], in0=xtb[:], scalar=ALPHA, in1=rr[:], op0=ALU.mult, op1=ALU.add),
                             reads=[xres] + [("rr", c) for c in range(4)], writes=[("rr", c) for c in range(4)])
                        layer_norm((rr[:], [("rr", c) for c in range(4)]), (x1b[:], x1res), (l1g[:], "l1g"), (l1b[:], "l1b"), (st6, mv), "ln1")
                        S.dma("sp", X1_d[t * 128:(t + 1) * 128, :], x1b[:], reads=[x1res], writes=[uq("X1_d")])
                    def stageBC(t):
                        x1b = x1[t % 2]
                        x1res = "x1_%d" % (t % 2)
                        for kc in range(16):
                            kb_ = 4 + (kc // 4) % 2
                            S.op("pe", lambda e, kc=kc, kb_=kb_, x1b=x1b: e.transpose(banks[kb_][:, (kc % 4) * 128:(kc % 4 + 1) * 128],
                                                                                   x1b[:, kc * 128:(kc + 1) * 128], ident_f[:]),
                                 reads=[x1res, "ident_f"], writes=[PS[kb_]])
                            if kc % 4 == 3:
                                for k2 in range(kc - 3, kc + 1):
                                    S.op("act", lambda e, k2=k2, kb_=kb_: e.activation(
                                        out=h2Tf[:, k2, :], in_=banks[kb_][:, (k2 % 4) * 128:(k2 % 4 + 1) * 128], func=AF.Identity,
                                        bias=modFM[:, 48 + k2, 0:1], scale=modFM[:, 64 + k2, 0:1]),
                                        reads=[PS[kb_], "modFM"], writes=[("h2Tf", k2 // 4)])
                        S.op("pool", lambda e, t=t: e.tensor_copy(out=h2Tb[:, :, t * 128:(t + 1) * 128], in_=h2Tf[:]),
                             reads=[("h2Tf", q_) for q_ in range(4)], writes=[("h2Tb", t)])
                        for kc in range(16):
                            mm(banks[6][:, 0:36], h2Tf[:, kc, :], wr[:, kc, :], kc == 0, kc == 15, [("h2Tf", kc // 4), "wr"], [PS[6]])

                    def stageBCchain(t):
                        R = "rt"
                        S.op("dve", lambda e: e.tensor_tensor(out=lg[:], in0=banks[6][:, 0:36], in1=brb[:], op=ALU.add), reads=[PS[6], "brb"], writes=["lg"])
                        S.op("dve", lambda e: e.reduce_max(out=rt[:, 0:1], in_=lg[:, 0:4], axis=AX.X), reads=["lg"], writes=[R])
                        S.op("dve", lambda e: e.tensor_scalar_mul(out=rt[:, 1:2], in0=rt[:, 0:1], scalar1=-1.0), reads=[R], writes=[R])
                        S.op("act", lambda e: e.activation(out=rt[:, 4:8], in_=lg[:, 0:4], func=AF.Exp, bias=rt[:, 1:2], scale=1.0), reads=["lg", R], writes=[R])
                        S.op("dve", lambda e: e.reduce_sum(out=rt[:, 2:3], in_=rt[:, 4:8], axis=AX.X), reads=[R], writes=[R])
                        S.op("dve", lambda e: e.reciprocal(out=rt[:, 3:4], in_=rt[:, 2:3]), reads=[R], writes=[R])
                        S.op("dve", lambda e: e.tensor_scalar(out=rt[:, 8:12], in0=lg[:, 0:4], scalar1=rt[:, 0:1], scalar2=None, op0=ALU.is_equal),
                             reads=["lg", R], writes=[R])
                        S.op("dve", lambda e: e.tensor_tensor(out=lm[:].rearrange("p (g j) -> p g j", j=8), in0=lg[:, 4:36].rearrange("p (g j) -> p g j", j=8),
                                                              in1=rt[:, 8:12].unsqueeze(2).to_broadcast([128, 4, 8]), op=ALU.mult),
                             reads=["lg", R], writes=["lm"])
                        S.op("dve", lambda e: e.tensor_reduce(out=rt[:, 16:24], in_=lm[:].rearrange("p (g j) -> p j g", j=8), axis=AX.X, op=ALU.add),
                             reads=["lm"], writes=[R])
                        S.op("dve", lambda e: e.max(out=m8[:], in_=rt[:, 16:24]), reads=[R], writes=["m8"])
                        S.op("dve", lambda e: e.tensor_tensor(out=rt[:, 24:25], in0=m8[:, 1:2], in1=m8[:, 0:1], op=ALU.subtract), reads=["m8"], writes=[R])
                        S.op("act", lambda e: e.activation(out=rt[:, 25:26], in_=rt[:, 24:25], func=AF.Exp), reads=[R], writes=[R])
                        S.op("dve", lambda e: e.tensor_scalar_add(out=rt[:, 26:27], in0=rt[:, 25:26], scalar1=1.0), reads=[R], writes=[R])
                        S.op("dve", lambda e: e.reciprocal(out=rt[:, 27:28], in_=rt[:, 26:27]), reads=[R], writes=[R])
                        S.op("dve", lambda e: e.tensor_tensor(out=rt[:, 28:29], in0=rt[:, 25:26], in1=rt[:, 27:28], op=ALU.mult), reads=[R], writes=[R])
                        S.op("dve", lambda e: e.tensor_scalar(out=rt[:, 29:31], in0=rt[:, 27:29], scalar1=rt[:, 3:4], scalar2=None, op0=ALU.mult),
                             reads=[R], writes=[R])
                        S.op("dve", lambda e: e.tensor_scalar(out=s1[:], in0=lm[:], scalar1=m8[:, 0:1], scalar2=rt[:, 29:30], op0=ALU.is_equal, op1=ALU.mult),
                             reads=["lm", "m8", R], writes=["s1"])
                        S.op("dve", lambda e: e.tensor_scalar(out=s2[:], in0=lm[:], scalar1=m8[:, 1:2], scalar2=rt[:, 30:31], op0=ALU.is_equal, op1=ALU.mult),
                             reads=["lm", "m8", R], writes=["s2"])
                        S.op("dve", lambda e: e.tensor_tensor(out=s1[:], in0=s1[:], in1=s2[:], op=ALU.add), reads=["s1", "s2"], writes=["s1"])
                        S.op("dve", lambda e: e.tensor_tensor(out=s1[:].rearrange("p (g j) -> p g j", j=8), in0=s1[:].rearrange("p (g j) -> p g j", j=8),
                                                              in1=rt[:, 8:12].unsqueeze(2).to_broadcast([128, 4, 8]), op=ALU.mult),
                             reads=["s1", R], writes=["s1"])
                    def stageBC2(t):
                        S.op("pe", lambda e: e.transpose(banks[7][0:32, 0:128], s1[:], ident_f[:]), reads=["s1", "ident_f"], writes=[PS[7]])
                        S.op("act", lambda e: e.activation(out=wtT[:], in_=banks[7][0:32, 0:128], func=AF.Copy), reads=[PS[7]], writes=["wtT"])
                        S.dma("sp", Wt_d[:, t * 128:(t + 1) * 128], wtT[:], reads=["wtT"], writes=[uq("Wt_d")])
                    load5(0)
                    load5(1)
                    stageA(0)
                    stageA2(0)
                    stageA(1)
                    for t in range(8):
                        if t + 2 < 8:
                            load5(t + 2)
                        stageBC(t)
                        if t + 1 < 8:
                            stageA2(t + 1)
                        stageBCchain(t)
                        if t + 2 < 8:
                            stageA(t + 2)
                        stageBC2(t)
                    S.flush()

            with contextlib.ExitStack() as ph67:
                yacc = sbt(ph67, "yacc", [128, 8, D], F32)
                with contextlib.ExitStack() as ph:
                    ring = [sbt(ph, "ring%d" % i, [128, 8192], BF16) for i in range(4)]
                    wtb = [sbt(ph, "wtb%d" % i, [128, OWN], F32) for i in range(2)]
                    sg = [sbt(ph, "sg%d" % i, [128, 512], F32) for i in range(2)]
                    tm = [sbt(ph, "tm%d" % i, [128, 512], F32) for i in range(2)]
                    actT = [sbt(ph, "actT%d" % i, [128, 4, 512], BF16) for i in range(2)]
                    cntR = 0
                    cntD = 0
                    for ei in range(n_exp):
                        slots = []
                        for wi, src in enumerate((wg_p, wu_p, wd_p)):
                            k = cntR % 4
                            cntR += 1
                            S.dma("pool", ring[k][:], src[ei].rearrange("p a b -> p (a b)"), writes=["ring%d" % k])
                            slots.append(k)
                        Gt = ring[slots[0]][:].rearrange("p (a b) -> p a b", b=512)
                        Ut = ring[slots[1]][:].rearrange("p (a b) -> p a b", b=512)
                        Dt = ring[slots[2]][:].rearrange("p (a b) -> p a b", b=D)
                        gres, ures, dres = ["ring%d" % k for k in slots]
                        wb = wtb[ei % 2]
                        wbres = "wtb%d" % (ei % 2)
                        S.dma("sp", wb[:], Wt_d[ei:ei + 1, :].partition_broadcast(128), writes=[wbres])
                        for tt in range(2):
                            for c in range(4):
                                kg, ku = c % 2, 2 + c % 2
                                for kc in range(16):
                                    mm(banks[kg][:], Gt[:, kc, c * 128:(c + 1) * 128], h2Tb[:, kc, tt * 512:(tt + 1) * 512], kc == 0, kc == 15,
                                       [gres, "h2Tb"], [PS[kg]])
                                for kc in range(16):
                                    mm(banks[ku][:], Ut[:, kc, c * 128:(c + 1) * 128], h2Tb[:, kc, tt * 512:(tt + 1) * 512], kc == 0, kc == 15,
                                       [ures, "h2Tb"], [PS[ku]])
                                sgb, tmb = sg[c % 2], tm[c % 2]
                                S.op("act", lambda e, sgb=sgb, kg=kg: e.activation(out=sgb[:], in_=banks[kg][:], func=AF.Silu),
                                     reads=[PS[kg]], writes=["sg%d" % (c % 2)])
                                S.op("dve", lambda e, sgb=sgb, tmb=tmb, ku=ku: e.tensor_tensor(out=tmb[:], in0=sgb[:], in1=banks[ku][:], op=ALU.mult),
                                     reads=["sg%d" % (c % 2), PS[ku]], writes=["tm%d" % (c % 2)])
                                S.op("dve", lambda e, tmb=tmb, tt=tt, c=c, wb=wb: e.tensor_tensor(out=actT[tt][:, c, :], in0=tmb[:],
                                                                                           in1=wb[:, tt * 512:(tt + 1) * 512], op=ALU.mult),
                                     reads=["tm%d" % (c % 2), wbres], writes=[("actT", tt, c)])
                        for tt in range(2):
                            for ts in range(4):
                                tile = tt * 4 + ts
                                for n4 in range(4):
                                    kd = 4 + cntD % 4
                                    cntD += 1
                                    for c in range(4):
                                        mm(banks[kd][:], actT[tt][:, c, ts * 128:(ts + 1) * 128], Dt[:, c, n4 * 512:(n4 + 1) * 512], c == 0, c == 3,
                                           [("actT", tt, c), dres], [PS[kd]])
                                    yres = ("yacc", tile, n4)
                                    if ei == 0:
                                        S.op("act", lambda e, kd=kd, tile=tile, n4=n4: e.activation(out=yacc[:, tile, n4 * 512:(n4 + 1) * 512],
                                                                                               in_=banks[kd][:], func=AF.Copy),
                                             reads=[PS[kd]], writes=[yres])
                                    else:
                                        S.op("dve", lambda e, kd=kd, tile=tile, n4=n4: e.tensor_tensor(
                                            out=yacc[:, tile, n4 * 512:(n4 + 1) * 512], in0=banks[kd][:],
                                            in1=yacc[:, tile, n4 * 512:(n4 + 1) * 512], op=ALU.add),
                                            reads=[PS[kd], yres], writes=[yres])
                    S.flush()

                with contextlib.ExitStack() as ph:
                    G2B = sbt(ph, "G2B", [128, D], F32)
                    l2g = sbt(ph, "l2g", [128, D], F32)
                    l2b = sbt(ph, "l2b", [128, D], F32)
                    x1t = [sbt(ph, "x1t%d" % i, [128, D], F32) for i in range(2)]
                    ot = [sbt(ph, "ot%d" % i, [128, D], F32) for i in range(2)]
                    st6b = sbt(ph, "st6b", [128, 4, 6], F32)
                    mvb = sbt(ph, "mvb", [128, 4], F32)
                    S.dma("sp", G2B[:], G_d[1], writes=["G2B"])
                    S.dma("sp", l2g[:], ln2g_in.partition_broadcast(128), writes=["l2g"])
                    S.dma("sp", l2b[:], ln2b_in.partition_broadcast(128), writes=["l2b"])
                    outs = []
                    S.dma("sp", x1t[0][:], X1_d[0:128, :], writes=["x1t0"])
                    for t in range(8):
                        xb_, ob_ = x1t[t % 2], ot[t % 2]
                        xres, ores = "x1t%d" % (t % 2), "ot%d" % (t % 2)
                        if t + 1 < 8:
                            S.dma("sp", x1t[(t + 1) % 2][:], X1_d[(t + 1) * 128:(t + 2) * 128, :], writes=["x1t%d" % ((t + 1) % 2)])
                        yv = yacc[:, t, :]
                        S.op("dve", lambda e, yv=yv: e.tensor_tensor(out=yv, in0=yv, in1=G2B[:], op=ALU.mult), reads=["G2B"], writes=[("y", t)])
                        S.op("dve", lambda e, yv=yv, xb_=xb_: e.scalar_tensor_tensor(out=yv, in0=xb_[:], scalar=ALPHA, in1=yv, op0=ALU.mult, op1=ALU.add),
                             reads=[xres, ("y", t)], writes=[("y", t)])
                        layer_norm((yv, ("y", t)), (ob_[:], ores), (l2g[:], "l2g"), (l2b[:], "l2b"), (st6b, mvb), "ln2")
                        S.dma("sp", out[t * 128:(t + 1) * 128, :], ob_[:], reads=[ores], writes=[("out", t)])
                        outs.append(("out", t))
                    S.op("sp", None, reads=outs)
                    S.flush()
    return nc


def _core_perm(q):
    own = list(range(q * 8, q * 8 + 8))
    prev = q * 8 - 1 if q > 0 else 31
    nxt = q * 8 + 8 if q < 3 else 0
    rest = [b for b in range(32) if b not in own and b != prev and b != nxt]
    blocks = own + [prev, nxt] + rest
    perm = np.concatenate([np.arange(b * 128, (b + 1) * 128) for b in blocks])
    return perm


def _rope_tables(perm):
    row = (perm // 64).astype(np.float32)
    col = (perm % 64).astype(np.float32)
    inv_freq = (10000.0 ** (-np.arange(32, dtype=np.float32) / np.float32(32))).astype(np.float32)
    ang_r = (row[None, :] * inv_freq[:, None]).astype(np.float32)
    ang_c = (col[None, :] * inv_freq[:, None]).astype(np.float32)
    C = np.concatenate([np.cos(ang_r), np.cos(ang_r), np.cos(ang_c), np.cos(ang_c)], axis=0).astype(np.float32)
    Sg = np.concatenate([-np.sin(ang_r), np.sin(ang_r), -np.sin(ang_c), np.sin(ang_c)], axis=0).astype(np.float32)
    return np.ascontiguousarray(C), np.ascontiguousarray(Sg)


def _prep_shared(inp):
    f = lambda a: np.ascontiguousarray(np.asarray(a, dtype=np.float32))
    sh = {}
    w_ada = f(inp["w_ada"])[0]
    sh["w_ada_p"] = np.ascontiguousarray(w_ada.reshape(16, 128, 24, 512).transpose(2, 1, 0, 3))
    b_ada = f(inp["b_ada"])[0]
    sh["b_ada_fm"] = np.ascontiguousarray(b_ada.reshape(96, 128).T)
    sh["b_ada_row"] = np.ascontiguousarray(b_ada.reshape(1, -1))
    w_in = f(inp["w_in"])[0]
    sh["w_in_p"] = np.ascontiguousarray(w_in.reshape(16, 128, 9, 512).transpose(2, 1, 0, 3))
    w_out = f(inp["w_out"])[0]
    sh["w_out_p"] = np.ascontiguousarray(w_out.reshape(16, 128, D).transpose(1, 0, 2))
    w_r = np.concatenate([f(inp["w_router_group"])[0], f(inp["w_router_expert"])[0]], axis=1)
    sh["w_r"] = np.ascontiguousarray(w_r.reshape(16, 128, 36).transpose(1, 0, 2))
    sh["b_r"] = np.concatenate([f(inp["b_router_group"])[0], f(inp["b_router_expert"])[0]]).reshape(1, 36)
    sh["wg_p"] = np.ascontiguousarray(f(inp["w_gate"])[0].reshape(NEXP, 16, 128, 512).transpose(0, 2, 1, 3))
    sh["wu_p"] = np.ascontiguousarray(f(inp["w_up"])[0].reshape(NEXP, 16, 128, 512).transpose(0, 2, 1, 3))
    sh["wd_p"] = np.ascontiguousarray(f(inp["w_down"])[0].reshape(NEXP, 4, 128, D).transpose(0, 2, 1, 3))
    sh["sink"] = f(inp["sink"]).reshape(1, 8)
    sh["lamv"] = np.concatenate([f(inp[k])[0] for k in ("lam_q1", "lam_k1", "lam_q2", "lam_k2")]).reshape(1, 512)
    sh["subln_g"] = f(inp["subln_g"]).reshape(1, 256)
    for k in ("ln1_g", "ln1_b", "ln2_g", "ln2_b"):
        sh[k] = f(inp[k]).reshape(1, D)
    sh["ident_b"] = np.eye(128, dtype=np.float32).astype(ml_dtypes.bfloat16)
    sh["ident_f"] = np.eye(128, dtype=np.float32)
    partner = np.concatenate([np.arange(32, 64), np.arange(0, 32), np.arange(96, 128), np.arange(64, 96)])
    psw = np.zeros((128, 128), np.float32)
    psw[partner, np.arange(128)] = 1.0
    sh["pswap"] = psw.astype(ml_dtypes.bfloat16)
    return sh


def _prep_core(inp, sh, core):
    b, q = core // 4, core % 4
    perm = _core_perm(q)
    m = dict(sh)
    x = np.asarray(inp["x"], dtype=np.float32)
    m["xs"] = np.ascontiguousarray(x[b][perm])
    m["ctxs"] = np.ascontiguousarray(np.asarray(inp["ctx"], dtype=np.float32)[b])
    cvec = np.stack([np.asarray(inp["c"], dtype=np.float32)[b], np.asarray(inp["c_ctx"], dtype=np.float32)], axis=1)
    m["cs"] = np.ascontiguousarray(cvec.reshape(16, 128, 2).transpose(1, 0, 2))
    C, Sg = _rope_tables(perm)
    m["ropeC"], m["ropeS"] = C, Sg
    jj = np.arange(128)[:, None]
    ii = np.arange(128)[None, :]
    tri_prev = (jj >= ii).astype(np.float32)
    tri_next = (jj <= ii).astype(np.float32)
    z = np.zeros((128, 128), np.float32)
    masks = np.stack([tri_prev, tri_next, tri_prev if q > 0 else z, tri_next if q < 3 else z], axis=1)
    m["masks"] = np.ascontiguousarray(masks).astype(ml_dtypes.bfloat16)
    return m


_NC_CACHE = {}


def kernel(**inputs):
    sh = _prep_shared(inputs)
    in_maps = [_prep_core(inputs, sh, c) for c in range(8)]
    if "nc" not in _NC_CACHE:
        _NC_CACHE["nc"] = build()
    nc = _NC_CACHE["nc"]
    res = run_bass_kernel_spmd(nc, in_maps, core_ids=list(range(8)))
    out = np.zeros((2, SEQ, D), np.float32)
    for c in range(8):
        b, q = c // 4, c % 4
        out[b, q * OWN:(q + 1) * OWN] = np.asarray(res.results[c]["out"], dtype=np.float32)
    return out
```
